# Optimizing a Trainium2 kernel written in Bass

```python
import math
import jax, jax.numpy as jnp
from jax import lax
import numpy as np

D_MODEL = 2048
BATCH = 2
SEQ = 8192
DEPTH = 1

CTX_LEN = 256
GRID_W = 64
CONV_WIDTH = 1024
CONV_K = 3
MLA_HEADS = 8
QK_NOPE = 128
QK_ROPE = 64
V_DIM = 128
Q_LORA = 512
KV_LORA = 512
MLA_WIDTH = MLA_HEADS * V_DIM
MIX_WIDTH = CONV_WIDTH + MLA_WIDTH
IN_COLS = 3 * CONV_WIDTH + Q_LORA + KV_LORA + QK_ROPE
N_EXPERTS = 16
EXPERT_FF = 1408
EC_FACTOR = 2
Q_BLOCK = 128
ROPE_THETA = 10000.0
ROPE_PAIRS = QK_ROPE // 4
ATTN_SCALE = 1.0 / math.sqrt(QK_NOPE + QK_ROPE)
EPS = 1e-6

kernel_name = "hybrid_conv_mla_ec_moe_dit"


def rmsnorm(h, g):
    h32 = h.astype(jnp.float32)
    h32 = h32 * lax.rsqrt(jnp.mean(h32 * h32, axis=-1, keepdims=True) + EPS)
    return h32.astype(h.dtype) * g


def modulate(h, shift, scale):
    return h * (1 + scale) + shift


def rotate_pairs(xa, ang):
    x1, x2 = jnp.split(xa, 2, axis=-1)
    cos, sin = jnp.cos(ang), jnp.sin(ang)
    return jnp.concatenate([x1 * cos - x2 * sin, x1 * sin + x2 * cos], axis=-1)


def rope2d(x, ang_row, ang_col):
    x32 = x.astype(jnp.float32)
    half = QK_ROPE // 2
    out = jnp.concatenate([rotate_pairs(x32[..., :half], ang_row),
                           rotate_pairs(x32[..., half:], ang_col)], axis=-1)
    return out.astype(x.dtype)


def dwconv3(u, w):
    up = jnp.pad(u, ((0, 0), (1, 1), (0, 0)))
    return up[:, :-2] * w[0] + up[:, 1:-1] * w[1] + up[:, 2:] * w[2]


def short_conv_mixer(p, w):
    xin = p[..., :CONV_WIDTH]
    bg = p[..., CONV_WIDTH:2 * CONV_WIDTH]
    cg = p[..., 2 * CONV_WIDTH:3 * CONV_WIDTH]
    return bg * dwconv3(cg * xin, w)


def mla_latents(p):
    o = 3 * CONV_WIDTH
    cq = p[..., o:o + Q_LORA]
    ckv = p[..., o + Q_LORA:o + Q_LORA + KV_LORA]
    kr = p[..., o + Q_LORA + KV_LORA:]
    return cq, ckv, kr


def mla_queries(cq, q_g, w_uq):
    q = jnp.einsum('bsr,rhd->bshd', rmsnorm(cq, q_g), w_uq)
    return q[..., :QK_NOPE], q[..., QK_NOPE:]


def mla_kv(ckv, kv_g, w_ukv):
    kv = jnp.einsum('bsr,rhd->bshd', rmsnorm(ckv, kv_g), w_ukv)
    return kv[..., :QK_NOPE], kv[..., QK_NOPE:]


def mla_attend(qn, qr, kn, kr, v):
    s = jnp.einsum('bqhd,bkhd->bhqk', qn, kn) + jnp.einsum('bqhr,bkr->bhqk', qr, kr)
    p = jax.nn.softmax(s.astype(jnp.float32) * ATTN_SCALE, axis=-1).astype(v.dtype)
    return jnp.einsum('bhqk,bkhd->bqhd', p, v)


def blocked_attention(qn, qr, kn, kr, v):
    b, s, h, _ = qn.shape
    nblk = s // Q_BLOCK
    qn_b = qn.reshape(b, nblk, Q_BLOCK, h, QK_NOPE).transpose(1, 0, 2, 3, 4)
    qr_b = qr.reshape(b, nblk, Q_BLOCK, h, QK_ROPE).transpose(1, 0, 2, 3, 4)
    o = lax.map(lambda a: mla_attend(a[0], a[1], kn, kr, v), (qn_b, qr_b))
    return o.transpose(1, 0, 2, 3, 4).reshape(b, s, h * V_DIM)


def expert_choice_ffn(h, w_router, w_gate, w_up, w_down):
    b, n, d = h.shape
    cap = EC_FACTOR * n // N_EXPERTS
    logits = jnp.einsum('bnd,de->bne', h, w_router).astype(jnp.float32)
    aff = jax.nn.softmax(logits, axis=-1)
    g, idx = lax.top_k(jnp.swapaxes(aff, 1, 2), cap)
    idx_flat = idx.reshape(b, N_EXPERTS * cap)
    xs = jax.vmap(lambda hb, ib: hb[ib])(h, idx_flat).reshape(b, N_EXPERTS, cap, d)
    hid = jax.nn.silu(jnp.einsum('becd,edf->becf', xs, w_gate)) * jnp.einsum('becd,edf->becf', xs, w_up)
    y = jnp.einsum('becf,efd->becd', hid, w_down) * g[..., None].astype(h.dtype)
    y = y.reshape(b, N_EXPERTS * cap, d)
    return jax.vmap(lambda ib, yb: jnp.zeros((n, d), h.dtype).at[ib].add(yb))(idx_flat, y)


def setup_inputs(seed: int = 0) -> dict:
    key = jax.random.key(seed)
    ks = jax.random.split(key, 24)
    f32 = jnp.float32
    nrm = lambda k, shape, s: jax.random.normal(k, shape, f32) * s
    return {
        "x": nrm(ks[0], (BATCH, SEQ, D_MODEL), 1.0),
        "c": nrm(ks[1], (BATCH, D_MODEL), 1.0),
        "ctx": nrm(ks[2], (BATCH, CTX_LEN, D_MODEL), 1.0),
        "c_ctx": nrm(ks[3], (D_MODEL,), 1.0),
        "w_mod": nrm(ks[4], (DEPTH, D_MODEL, 6 * D_MODEL), 0.5 * D_MODEL ** -0.5),
        "b_mod": nrm(ks[5], (DEPTH, 6 * D_MODEL), 0.01),
        "norm1_g": 1.0 + nrm(ks[6], (DEPTH, D_MODEL), 0.02),
        "norm2_g": 1.0 + nrm(ks[7], (DEPTH, D_MODEL), 0.02),
        "w_in": nrm(ks[8], (DEPTH, D_MODEL, IN_COLS), D_MODEL ** -0.5),
        "conv_w": nrm(ks[9], (DEPTH, CONV_K, CONV_WIDTH), CONV_K ** -0.5),
        "q_norm_g": 1.0 + nrm(ks[10], (DEPTH, Q_LORA), 0.02),
        "w_uq": nrm(ks[11], (DEPTH, Q_LORA, MLA_HEADS, QK_NOPE + QK_ROPE), Q_LORA ** -0.5),
        "kv_norm_g": 1.0 + nrm(ks[12], (DEPTH, KV_LORA), 0.02),
        "w_ukv": nrm(ks[13], (DEPTH, KV_LORA, MLA_HEADS, QK_NOPE + V_DIM), KV_LORA ** -0.5),
        "w_out": nrm(ks[14], (DEPTH, MIX_WIDTH, D_MODEL), MIX_WIDTH ** -0.5),
        "w_router": nrm(ks[15], (DEPTH, D_MODEL, N_EXPERTS), D_MODEL ** -0.5),
        "w_gate": nrm(ks[16], (DEPTH, N_EXPERTS, D_MODEL, EXPERT_FF), D_MODEL ** -0.5),
        "w_up": nrm(ks[17], (DEPTH, N_EXPERTS, D_MODEL, EXPERT_FF), D_MODEL ** -0.5),
        "w_down": nrm(ks[18], (DEPTH, N_EXPERTS, EXPERT_FF, D_MODEL), EXPERT_FF ** -0.5),
        "final_g": 1.0 + nrm(ks[19], (D_MODEL,), 0.02),
    }


def reference(x, c, ctx, c_ctx, w_mod, b_mod, norm1_g, norm2_g, w_in, conv_w, q_norm_g, w_uq,
              kv_norm_g, w_ukv, w_out, w_router, w_gate, w_up, w_down, final_g):
    s = x.shape[1]
    n_rows = s // GRID_W
    row = jnp.repeat(jnp.arange(n_rows), GRID_W).astype(jnp.float32)
    col = jnp.tile(jnp.arange(GRID_W), n_rows).astype(jnp.float32)
    inv_freq = ROPE_THETA ** (-jnp.arange(ROPE_PAIRS, dtype=jnp.float32) / ROPE_PAIRS)
    ang_r = row[:, None] * inv_freq[None, :]
    ang_c = col[:, None] * inv_freq[None, :]

    for l in range(DEPTH):
        mod = jax.nn.silu(c) @ w_mod[l] + b_mod[l]
        mod_c = jax.nn.silu(c_ctx) @ w_mod[l] + b_mod[l]
        sh1, sc1, g1, sh2, sc2, g2 = jnp.split(mod[:, None, :], 6, axis=-1)
        shc1, scc1, gc1, shc2, scc2, gc2 = jnp.split(mod_c, 6, axis=-1)

        h = modulate(rmsnorm(x, norm1_g[l]), sh1, sc1)
        hc = modulate(rmsnorm(ctx, norm1_g[l]), shc1, scc1)
        p = h @ w_in[l]
        pc = hc @ w_in[l]

        y_conv = short_conv_mixer(p, conv_w[l])

        cq, ckv, kr = mla_latents(p)
        cq_c, ckv_c, kr_c = mla_latents(pc)
        q_nope, q_rope = mla_queries(cq, q_norm_g[l], w_uq[l])
        q_rope = rope2d(q_rope, ang_r[:, None, :], ang_c[:, None, :])
        k_nope, v = mla_kv(ckv, kv_norm_g[l], w_ukv[l])
        k_rope = rope2d(kr, ang_r, ang_c)
        kc_nope, vc = mla_kv(ckv_c, kv_norm_g[l], w_ukv[l])
        kn_all = jnp.concatenate([k_nope, kc_nope], axis=1)
        kr_all = jnp.concatenate([k_rope, kr_c], axis=1)
        v_all = jnp.concatenate([v, vc], axis=1)
        y_attn = blocked_attention(q_nope, q_rope, kn_all, kr_all, v_all)

        y = jnp.concatenate([y_conv, y_attn], axis=-1) @ w_out[l]
        x = x + g1 * y

        if l < DEPTH - 1:
            yc_conv = short_conv_mixer(pc, conv_w[l])
            qc_nope, qc_rope = mla_queries(cq_c, q_norm_g[l], w_uq[l])
            b_, lc = ctx.shape[0], ctx.shape[1]
            yc_attn = mla_attend(qc_nope, qc_rope, kc_nope, kr_c, vc).reshape(b_, lc, MLA_WIDTH)
            ctx = ctx + gc1 * (jnp.concatenate([yc_conv, yc_attn], axis=-1) @ w_out[l])
            hc2 = modulate(rmsnorm(ctx, norm2_g[l]), shc2, scc2)
            ctx = ctx + gc2 * expert_choice_ffn(hc2, w_router[l], w_gate[l], w_up[l], w_down[l])

        h2 = modulate(rmsnorm(x, norm2_g[l]), sh2, sc2)
        x = x + g2 * expert_choice_ffn(h2, w_router[l], w_gate[l], w_up[l], w_down[l])

    return rmsnorm(x, final_g)
```

```python
import contextlib
import math
import numpy as np
import concourse.bass as bass
import concourse.mybir as mybir
from concourse.bass_utils import run_bass_kernel_spmd

F32 = mybir.dt.float32
BF16 = mybir.dt.bfloat16
ALU = mybir.AluOpType
AF = mybir.ActivationFunctionType
AX = mybir.AxisListType

D = 2048
NOWN = 2048
NOTH = 6144
NCTX = 256
NKEY = NOWN + NOTH + NCTX
NE = 16
FF = 1408
NF = 11
CAP = 1024
EPS = 1e-6
SCALE = 1.0 / math.sqrt(192.0)
THR_ITERS = 30
CSLOT = 384


class Prog:
    def __init__(self, nc, stack, dma_ring=6):
        self.nc = nc
        names = ["pe", "act", "dve", "pool", "sp"]
        self.sem = {k: stack.enter_context(nc.semaphore("s_" + k)) for k in names}
        self.cnt = {k: 0 for k in names}
        self.lists = {k: [] for k in names}
        self.waited = {k: {} for k in names}
        self.ring = {}
        self.ringn = {}
        for q in ("sp", "pool"):
            self.ring[q] = [stack.enter_context(nc.semaphore(f"d_{q}{i}")) for i in range(dma_ring)]
            self.ringn[q] = 0
        self.csems = [stack.enter_context(nc.semaphore(f"c_{i}")) for i in range(10)]
        self.ncoll = 0
        self.lastw = {}
        self.readers = {}

    def coll(self, fn, reads=(), writes=()):
        reads = list(reads); writes = list(writes)
        sem = self.csems[self.ncoll]; self.ncoll += 1
        waits = self._waits("pool", self._deps(reads, writes))
        self.lists["pool"].append((waits, fn, sem, 1))
        self._record((sem, 1, "coll"), reads, writes)

    def _deps(self, reads, writes):
        deps = []
        for r in reads:
            if r in self.lastw:
                deps.append(self.lastw[r])
        for w in writes:
            if w in self.lastw:
                deps.append(self.lastw[w])
            deps.extend(self.readers.get(w, []))
        return deps

    def _waits(self, e, deps):
        out = []
        for (sem, val, src) in deps:
            if src == e and e == "pe":
                continue
            key = id(sem)
            if self.waited[e].get(key, -1) >= val:
                continue
            self.waited[e][key] = val
            out.append((sem, val))
        return out

    def _record(self, tok, reads, writes):
        for w in writes:
            self.lastw[w] = tok
            self.readers[w] = []
        for r in reads:
            if r not in writes:
                self.readers.setdefault(r, []).append(tok)

    def op(self, e, fn, reads=(), writes=(), inc=True):
        reads = list(reads); writes = list(writes)
        waits = self._waits(e, self._deps(reads, writes))
        sem = self.sem[e]
        if inc:
            self.cnt[e] += 1
            n = self.cnt[e]
        else:
            n = self.cnt[e] + 1
        self.lists[e].append((waits, fn, sem, 1 if inc else 0))
        self._record((sem, n, e), reads, writes)

    def dma(self, q, out, in_, reads=(), writes=(), **kw):
        reads = list(reads); writes = list(writes)
        j = self.ringn[q]; self.ringn[q] += 1
        S = len(self.ring[q])
        sem = self.ring[q][j % S]
        tgt = 16 * (j // S + 1)
        deps = self._deps(reads, writes)
        if j >= S:
            deps.append((sem, tgt - 16, "dma"))
        waits = self._waits(q, deps)
        fn = lambda eng, out=out, in_=in_, kw=kw: eng.dma_start(out=out, in_=in_, **kw)
        self.lists[q].append((waits, fn, sem, 16))
        self._record((sem, tgt, "dma"), reads, writes)

    def dma_fn(self, q, fn, reads=(), writes=()):
        reads = list(reads); writes = list(writes)
        j = self.ringn[q]; self.ringn[q] += 1
        S = len(self.ring[q])
        sem = self.ring[q][j % S]
        tgt = 16 * (j // S + 1)
        deps = self._deps(reads, writes)
        if j >= S:
            deps.append((sem, tgt - 16, "dma"))
        waits = self._waits(q, deps)
        self.lists[q].append((waits, fn, sem, 16))
        self._record((sem, tgt, "dma"), reads, writes)

    def barrier(self):
        deps = []
        for e in self.cnt:
            if self.cnt[e] > 0:
                deps.append((self.sem[e], self.cnt[e], "x"))
        for q in self.ring:
            S = len(self.ring[q]); n = self.ringn[q]
            for i in range(S):
                c = (n - i + S - 1) // S if n > i else 0
                if c > 0:
                    deps.append((self.ring[q][i], 16 * c, "dma"))
        for i in range(self.ncoll):
            deps.append((self.csems[i], 1, "coll"))
        for e in self.lists:
            waits = self._waits(e, [d for d in deps if d[2] != e or e != "pe"])
            self.lists[e].append((waits, None, None, 0))
        self.lastw = {}
        self.readers = {}

    def run(self):
        with self.nc.Block() as block:
            def mk(e):
                def body(eng):
                    for (waits, fn, sem, inc) in self.lists[e]:
                        for (s, v) in waits:
                            eng.wait_ge(s, v)
                        if fn is not None:
                            ins = fn(eng)
                            if inc:
                                ins.then_inc(sem, inc)
                return body
            block.tensor(mk("pe"))
            block.scalar(mk("act"))
            block.vector(mk("dve"))
            block.gpsimd(mk("pool"))
            block.sync(mk("sp"))


def build(dbg=False, stop_after=99):
    nc = bass.Bass("TRN2", target_bir_lowering=False)
    in_names = []

    def dt_in(name, shape):
        in_names.append(name)
        return nc.dram_tensor(name, shape, F32, kind="ExternalInput").ap()
    nc.in_names = in_names
    x_own = dt_in("x_own", [NOWN, D])
    x_ctx = dt_in("x_ctx", [NCTX, D])
    x_halo = dt_in("x_halo", [2, D])
    hmask_d = dt_in("hmask", [128, 2])
    c_pk = dt_in("c_pk", [128, 32])
    cos_own_d = dt_in("cos_own", [64, NOWN]); sin_own_d = dt_in("sin_own", [64, NOWN])
    w_mod_d = dt_in("w_mod_q", [8, 128, 16 * 512])
    b_mod_d = dt_in("b_mod_q", [8, 512])
    n1g_d = dt_in("norm1_g", [1, D]); n2g_d = dt_in("norm2_g", [1, D]); fg_d = dt_in("final_g", [1, D])
    w_in_d = dt_in("w_in_r", [33, 128, 16 * 128])
    conv_d = dt_in("conv_r", [128, 24])
    qg_d = dt_in("qg_r", [128, 4]); kvg_d = dt_in("kvg_r", [128, 4])
    w_uq_d = dt_in("w_uq_r", [8, 128, 4 * 256]); w_ukv_d = dt_in("w_ukv_r", [8, 128, 4 * 256])
    w_out_d = dt_in("w_out_r", [128, 16 * D])
    w_rt_d = dt_in("w_rt_r", [128, 16 * NE])
    if stop_after >= 6:
        w_g_d = dt_in("w_g_r", [NE, NF, 128, 16 * 128]); w_u_d = dt_in("w_u_r", [NE, NF, 128, 16 * 128])
        w_d_d = dt_in("w_d_r", [NE, 4, 128, NF * 512])
    ident_d = dt_in("ident", [128, 128])
    gmat_d = dt_in("gmat", [128, 128])
    ltri_d = dt_in("ltri", [128, 128])
    iota_d = dt_in("iota", [128, CSLOT])
    out_d = nc.dram_tensor("out", [NOWN, D], F32, kind="ExternalOutput").ap()

    modrows = nc.dram_tensor("modrows", [6, D], F32).ap()
    mod_part = nc.dram_tensor("mod_part", [8, 512], F32).ap()
    mod_all = nc.dram_tensor("mod_all", [32, 512], F32).ap()
    st_ymix = nc.dram_tensor("st_ymix", [128, 16 * NOWN], BF16).ap()
    st_cqn = nc.dram_tensor("st_cqn", [128, 4 * NOWN], BF16).ap()
    st_ckvn_l = [nc.dram_tensor(f"st_ckvn{c}", [128, NOWN], BF16).ap() for c in range(4)]
    st_kr = nc.dram_tensor("st_kr", [64, NOWN], BF16).ap()
    st_x1 = nc.dram_tensor("st_x1", [NOWN, D], F32).ap()
    st_h2 = nc.dram_tensor("st_h2", [NOWN, D], BF16).ap()
    aff_in = nc.dram_tensor("aff_in", [16, NOWN], F32).ap()
    aff_out = nc.dram_tensor("aff_out", [64, NOWN], F32).ap()
    ag_ck_l = [nc.dram_tensor(f"ag_ck{c}", [4 * 128, NOWN], BF16).ap() for c in range(4)]
    ag_kr = nc.dram_tensor("ag_kr", [4 * 64, NOWN], BF16).ap()
    dbgout = {}
    if dbg:
        dbgout["d_modrows"] = nc.dram_tensor("d_modrows", [6, D], F32, kind="ExternalOutput").ap()
        dbgout["d_ymix"] = nc.dram_tensor("d_ymix", [128, 16 * NOWN], BF16, kind="ExternalOutput").ap()
        dbgout["d_cqn"] = nc.dram_tensor("d_cqn", [128, 4 * NOWN], BF16, kind="ExternalOutput").ap()
        dbgout["d_ckvn"] = nc.dram_tensor("d_ckvn", [128, 4 * NKEY], BF16, kind="ExternalOutput").ap()
        dbgout["d_kr"] = nc.dram_tensor("d_kr", [64, NKEY], BF16, kind="ExternalOutput").ap()
        dbgout["d_x1"] = nc.dram_tensor("d_x1", [NOWN, D], F32, kind="ExternalOutput").ap()
        dbgout["d_aff"] = nc.dram_tensor("d_aff", [64, NOWN], F32, kind="ExternalOutput").ap()
        dbgout["d_thr"] = nc.dram_tensor("d_thr", [64, 2], F32, kind="ExternalOutput").ap()

    with contextlib.ExitStack() as top:
        P = Prog(nc, top)
        uniq = [0]

        def sbt(st, name, shape, dt):
            uniq[0] += 1
            return st.enter_context(nc.sbuf_tensor(f"sb{uniq[0]}_{name}", shape, dt))
        tp = top.enter_context(nc.psum_tensor("tp", [128, 2, 8, 128], BF16))
        mm = [top.enter_context(nc.psum_tensor(f"mm{i}", [128, 512], F32)) for i in range(6)]
        ident_f = sbt(top, "ident_f", [128, 128], F32)
        ident_b = sbt(top, "ident_b", [128, 128], BF16)
        ones_f = sbt(top, "ones_f", [128, 128], F32)
        ones_b = sbt(top, "ones_b", [128, 128], BF16)
        stats = sbt(top, "stats", [128, 4 * 40], F32)
        afft = sbt(top, "afft", [128, 16, NE], F32)
        gmt = sbt(top, "gmt", [128, 16, NE], F32)
        mskp = sbt(top, "mskp", [128, 16, NE], F32)
        posf = sbt(top, "posf", [128, 16, NE], F32)
        P.dma("sp", ident_f[:], ident_d, writes=["ident_f"])
        P.op("dve", lambda e: e.tensor_copy(out=ident_b[:], in_=ident_f[:]), reads=["ident_f"], writes=["ident_b"])
        P.op("pool", lambda e: e.memset(ones_f[:], 1.0), writes=["ones_f"])
        P.op("pool", lambda e: e.memset(ones_b[:], 1.0), writes=["ones_b"])
        P.op("pool", lambda e: e.memset(stats[:], 0.0), writes=["stats"])
        stat_i = [0]

        def rstd_from_ss(ss_ap, n, key):
            i = stat_i[0]
            c1 = stats[0:ss_ap.shape[0], 4 * i + 1:4 * i + 2]
            c2 = stats[0:ss_ap.shape[0], 4 * i + 2:4 * i + 3]
            c3 = stats[0:ss_ap.shape[0], 4 * i + 3:4 * i + 4]
            P.op("dve", lambda e: e.tensor_scalar(out=c1, in0=ss_ap, scalar1=1.0 / n, scalar2=EPS, op0=ALU.mult, op1=ALU.add), reads=[key], writes=[key + "1"])
            P.op("act", lambda e: e.sqrt(out=c2, in_=c1), reads=[key + "1"], writes=[key + "2"])
            P.op("dve", lambda e: e.reciprocal(out=c3, in_=c2), reads=[key + "2"], writes=[key + "3"])
            return c3, key + "3"

        tile_ctr = [0]

        def tok_pipeline(xt, xt_key, hb, hb_key, A_t, A_key, sh_t, sh_key, dst_fn, dst_key, src=None, post=None, defer=False):
            i = stat_i[0] = (stat_i[0] + 1) % 40
            n = tile_ctr[0] = tile_ctr[0] + 1
            sk = f"st{i}"
            if src is not None:
                P.dma("sp", xt[:], src, writes=[xt_key])
            ss = stats[:, 4 * i:4 * i + 1]
            P.op("act", lambda e: e.activation(out=hb[:], in_=xt[:], func=AF.Square, accum_out=ss), reads=[xt_key], writes=[hb_key, sk])
            rs, rk = rstd_from_ss(ss, D, sk)
            P.op("dve", lambda e: e.scalar_tensor_tensor(out=xt[:], in0=xt[:], scalar=rs, in1=A_t[:], op0=ALU.mult, op1=ALU.mult), reads=[xt_key, rk, A_key], writes=[xt_key])
            P.op("pool", lambda e: e.tensor_tensor(out=hb[:], in0=xt[:], in1=sh_t[:], op=ALU.add), reads=[xt_key, sh_key], writes=[hb_key])
            if post is not None:
                post()

            def part2():
                for half in range(2):
                    for k in range(8):
                        kk = half * 8 + k
                        P.op("pe", lambda e, half=half, k=k, kk=kk: e.transpose(out=tp[:, half, k, :], in_=hb[:, kk * 128:(kk + 1) * 128], identity=ident_b[:]),
                             reads=[hb_key, "ident_b"], writes=[("tp", half)], inc=(k == 7))
                    dst = dst_fn(half)
                    if (n + half) % 2 == 0:
                        P.op("act", lambda e, half=half, dst=dst: e.copy(out=dst, in_=tp[:, half, :, :]), reads=[("tp", half)], writes=[dst_key])
                    else:
                        P.op("dve", lambda e, half=half, dst=dst: e.tensor_copy(out=dst, in_=tp[:, half, :, :]), reads=[("tp", half)], writes=[dst_key])

            if defer:
                return part2
            part2()

        mmrot = [0]
        mm_pool = [[0, 1, 2, 3, 4, 5]]

        def next_mm():
            mmrot[0] = (mmrot[0] + 1) % len(mm_pool[0])
            return mm_pool[0][mmrot[0]]

        def latent_norm(lhs_fn, rhs_fn, rkeys, gamma, N, out_fn, out_key, st):
            raw, sq, rr = st
            for ci in range(4):
                b_ = next_mm()
                for k in range(16):
                    P.op("pe", lambda e, b_=b_, ci=ci, k=k: e.matmul(out=mm[b_][:, 0:N], lhsT=lhs_fn(ci, k), rhs=rhs_fn(k), start=(k == 0), stop=(k == 15)),
                         reads=rkeys, writes=[("mm", b_)], inc=(k == 15))
                P.op("act", lambda e, b_=b_, ci=ci: e.copy(out=raw[:, ci, 0:N], in_=mm[b_][:, 0:N]), reads=[("mm", b_)], writes=[("raw", ci)])
                P.op("act", lambda e, ci=ci: e.activation(out=sq[:, ci, 0:N], in_=raw[:, ci, 0:N], func=AF.Square), reads=[("raw", ci)], writes=[("sq", ci)])
            b2 = next_mm()
            for ci in range(4):
                P.op("pe", lambda e, ci=ci: e.matmul(out=mm[b2][:, 0:N], lhsT=ones_b[:], rhs=sq[:, ci, 0:N], start=(ci == 0), stop=(ci == 3)),
                     reads=[("sq", ci), "ones_b"], writes=[("mm", b2)], inc=(ci == 3))
            P.op("dve", lambda e: e.tensor_scalar(out=rr[:, 0:N], in0=mm[b2][:, 0:N], scalar1=1.0 / 512, scalar2=EPS, op0=ALU.mult, op1=ALU.add), reads=[("mm", b2)], writes=["rr"])
            P.op("act", lambda e: e.sqrt(out=rr[:, 0:N], in_=rr[:, 0:N]), reads=["rr"], writes=["rr"])
            P.op("dve", lambda e: e.reciprocal(out=rr[:, 0:N], in_=rr[:, 0:N]), reads=["rr"], writes=["rr"])
            for ci in range(4):
                eng = "dve"
                P.op(eng, lambda e, ci=ci: e.scalar_tensor_tensor(out=out_fn(ci), in0=raw[:, ci, 0:N], scalar=gamma[:, ci:ci + 1], in1=rr[:, 0:N], op0=ALU.mult, op1=ALU.mult),
                     reads=[("raw", ci), "rr", "gam"], writes=[out_key])

        def rope_proj(lhs1_fn, lhs2_fn, rhs_fn, rkeys, nk, N, cos_t, sin_t, tkey, out_ap, out_key, tmp, do_rope=True):
            b1 = next_mm()
            for k in range(nk):
                P.op("pe", lambda e, k=k: e.matmul(out=mm[b1][0:64, 0:N], lhsT=lhs1_fn(k), rhs=rhs_fn(k), start=(k == 0), stop=(k == nk - 1)),
                     reads=rkeys, writes=[("mm", b1)], inc=(k == nk - 1))
            if not do_rope:
                P.op("act", lambda e: e.copy(out=out_ap, in_=mm[b1][0:64, 0:N]), reads=[("mm", b1)], writes=[out_key])
                return
            b2 = next_mm()
            for k in range(nk):
                P.op("pe", lambda e, k=k: e.matmul(out=mm[b2][0:64, 0:N], lhsT=lhs2_fn(k), rhs=rhs_fn(k), start=(k == 0), stop=(k == nk - 1)),
                     reads=rkeys, writes=[("mm", b2)], inc=(k == nk - 1))
            t1, t2 = tmp
            P.op("dve", lambda e: e.tensor_tensor(out=t1[0:64, 0:N], in0=mm[b1][0:64, 0:N], in1=cos_t, op=ALU.mult), reads=[("mm", b1), tkey], writes=["rt1"])
            P.op("dve", lambda e: e.tensor_tensor(out=t2[0:64, 0:N], in0=mm[b2][0:64, 0:N], in1=sin_t, op=ALU.mult), reads=[("mm", b2), tkey], writes=["rt2"])
            P.op("pool", lambda e: e.tensor_tensor(out=out_ap, in0=t1[0:64, 0:N], in1=t2[0:64, 0:N], op=ALU.add), reads=["rt1", "rt2"], writes=[out_key])


        def phase23(s12):
            A1, SH1 = s12.A1, s12.SH1
            with contextlib.ExitStack() as s23:
                ckvnT = sbt(s23, "ckvnT", [128, 4, NKEY], BF16)
                kropeT = sbt(s23, "kropeT", [128, NKEY], BF16)
                P.op("pool", lambda e: e.memset(kropeT[64:128, :], 0.0), writes=["kr_pad"])
                for r_ in range(4):
                    for c_ in range(4):
                        P.dma("sp", ckvnT[:, c_, r_ * NOWN:(r_ + 1) * NOWN], ag_ck_l[c_][r_ * 128:(r_ + 1) * 128, :],
                              reads=[("ag_ck", c_)], writes=[("ck", 4 * r_ + b_) for b_ in range(4)])
                    P.dma("sp", kropeT[0:64, r_ * NOWN:(r_ + 1) * NOWN], ag_kr[r_ * 64:(r_ + 1) * 64, :], reads=["ag_kr"], writes=[("kr", 4 * r_ + b_) for b_ in range(4)])
                with contextlib.ExitStack() as s2:
                    xt = [sbt(s2, f"xt{i}", [128, D], F32) for i in range(2)]
                    hb = [sbt(s2, f"hb{i}", [128, D], BF16) for i in range(2)]
                    xg = [sbt(s2, f"xg{i}", [128, 16, 512], BF16) for i in range(2)]
                    wkv = sbt(s2, "wkv", [128, 16, 640], BF16)
                    kvg = sbt(s2, "kvg", [128, 4], F32)
                    raw = sbt(s2, "raw", [128, 4, 512], F32)
                    sq = sbt(s2, "sq", [128, 4, 512], BF16)
                    rr = sbt(s2, "rr", [128, 512], F32)
                    rt1 = sbt(s2, "rt1", [64, 512], F32)
                    rt2 = sbt(s2, "rt2", [64, 512], F32)
                    cst = sbt(s2, "cst", [64, 2, 512], F32)
                    P.dma("sp", kvg[:], kvg_d, writes=["gam"])
                    for i in range(5):
                        P.dma("pool", wkv[:, :, i * 128:(i + 1) * 128], w_in_d[28 + i].rearrange("p (k n) -> p k n", n=128), writes=["wkv"])
                    for g in range(12, 13):
                        s_ = g % 2
                        n = 512 if g < 12 else NCTX
                        koff = NOWN + g * 512
                        blk = 4 + g
                        if g == 12:
                            P.dma("sp", A1[:], modrows[4:5, :].partition_broadcast(128), writes=["A1"])
                            P.dma("sp", SH1[:], modrows[5:6, :].partition_broadcast(128), writes=["SH1"])
                        for t in range(n // 128):
                            tt = g * 4 + t
                            xs = tt % 2
                            src = x_ctx[t * 128:(t + 1) * 128, :]
                            tok_pipeline(xt[xs], ("xt", xs), hb[xs], ("hb", xs), A1, "A1", SH1, "SH1",
                                         lambda half, t=t, s_=s_: xg[s_][:, half * 8:(half + 1) * 8, t * 128:(t + 1) * 128], ("xg", s_), src=src)
                        latent_norm(lambda ci, k: wkv[:, k, ci * 128:(ci + 1) * 128], lambda k, s_=s_, n=n: xg[s_][:, k, 0:n],
                                    ["wkv", ("xg", s_)], kvg, n,
                                    lambda ci, koff=koff, n=n: ckvnT[:, ci, koff:koff + n], ("ck", blk), (raw, sq, rr))
                        rope_proj(lambda k: wkv[:, k, 512:576], lambda k: wkv[:, k, 576:640], lambda k, s_=s_, n=n: xg[s_][:, k, 0:n],
                                  ["wkv", ("xg", s_)], 16, n, cst[:, 0, 0:n], cst[:, 1, 0:n], "cst", kropeT[0:64, koff:koff + n], ("kr", blk), (rt1, rt2), do_rope=(g < 12))
                P.barrier()
                if dbg:
                    P.dma("sp", dbgout["d_ckvn"].rearrange("p (c n) -> p c n", n=NKEY), ckvnT[:], writes=["d_ckvn"])
                    P.dma("sp", dbgout["d_kr"], kropeT[0:64, :], writes=["d_kr"])
                if stop_after < 3:
                    return
                with contextlib.ExitStack() as s3:
                    cqnT = sbt(s3, "cqnT", [128, 4, NOWN], BF16)
                    cosq = sbt(s3, "cosq", [64, NOWN], F32)
                    sinq = sbt(s3, "sinq", [64, NOWN], F32)
                    wq = [sbt(s3, f"wq{i}", [128, 4, 256], BF16) for i in range(2)]
                    wk = [sbt(s3, f"wk{i}", [128, 4, 256], BF16) for i in range(2)]
                    KT = sbt(s3, "KT", [128, NKEY], BF16)
                    V = sbt(s3, "V", [128, 66, 128], BF16)
                    QN = sbt(s3, "QN", [128, NOWN], BF16)
                    QR = sbt(s3, "QR", [128, NOWN], BF16)
                    P.op("pool", lambda e: e.memset(QR[64:128, :], 0.0), writes=["qr_pad"])
                    PT = [sbt(s3, f"PT{i}", [128, 512], BF16) for i in range(4)]
                    denA = sbt(s3, "denA", [128, 512], F32)
                    denB = sbt(s3, "denB", [128, 512], F32)
                    rden = sbt(s3, "rden", [128, 512], F32)
                    ya = [sbt(s3, f"ya{i}", [128, 512], BF16) for i in range(2)]
                    rt1 = sbt(s3, "rt1", [64, 512], F32)
                    rt2 = sbt(s3, "rt2", [64, 512], F32)
                    P.dma("sp", cqnT[:], st_cqn.rearrange("p (c n) -> p c n", n=NOWN), writes=["cqnT"])
                    P.dma("sp", cosq[:], cos_own_d, writes=["cosq"])
                    P.dma("sp", sinq[:], sin_own_d, writes=["cosq"])
                    mm_pool[0] = [4, 5]
                    ev = [0]

                    def evac(out_ap, in_ap, rk, wkey):
                        ev[0] += 1
                        if ev[0] % 2 == 0:
                            P.op("act", lambda e: e.copy(out=out_ap, in_=in_ap), reads=[rk], writes=[wkey])
                        else:
                            P.op("dve", lambda e: e.tensor_copy(out=out_ap, in_=in_ap), reads=[rk], writes=[wkey])

                    for h in range(8):
                        s_ = h % 2
                        P.dma("pool", wq[s_][:], w_uq_d[h].rearrange("p (k n) -> p k n", n=256), writes=[("wq", s_)])
                        P.dma("pool", wk[s_][:], w_ukv_d[h].rearrange("p (k n) -> p k n", n=256), writes=[("wk", s_)])
                        for nb in range(17):
                            n = 512 if nb < 16 else 256
                            b_ = next_mm()
                            for k in range(4):
                                P.op("pe", lambda e, b_=b_, k=k, nb=nb, n=n, s_=s_: e.matmul(out=mm[b_][:, 0:n], lhsT=wk[s_][:, k, 0:128], rhs=ckvnT[:, k, nb * 512:nb * 512 + n], start=(k == 0), stop=(k == 3)),
                                     reads=[("wk", s_), ("ck", nb)], writes=[("mm", b_)], inc=(k == 3))
                            evac(KT[:, nb * 512:nb * 512 + n], mm[b_][:, 0:n], ("mm", b_), ("KT", nb))
                        for vb in range(17):
                            nt = 4 if vb < 16 else 2
                            b_ = next_mm()
                            for i in range(nt):
                                kt = vb * 4 + i
                                for k in range(4):
                                    P.op("pe", lambda e, b_=b_, k=k, i=i, kt=kt, s_=s_: e.matmul(out=mm[b_][:, i * 128:(i + 1) * 128], lhsT=ckvnT[:, k, kt * 128:(kt + 1) * 128], rhs=wk[s_][:, k, 128:256], start=(k == 0), stop=(k == 3)),
                                         reads=[("wk", s_), ("ck", vb)], writes=[("mm", b_)], inc=(k == 3 and i == nt - 1))
                            evac(V[:, vb * 4:vb * 4 + nt, :], mm[b_][:, 0:nt * 128].rearrange("p (t d) -> p t d", d=128), ("mm", b_), ("V", vb))
                        for qb in range(4):
                            q0 = qb * 512
                            b_ = next_mm()
                            for k in range(4):
                                P.op("pe", lambda e, b_=b_, k=k, q0=q0, s_=s_: e.matmul(out=mm[b_][:], lhsT=wq[s_][:, k, 0:128], rhs=cqnT[:, k, q0:q0 + 512], start=(k == 0), stop=(k == 3)),
                                     reads=[("wq", s_), "cqnT"], writes=[("mm", b_)], inc=(k == 3))
                            evac(QN[:, q0:q0 + 512], mm[b_][:], ("mm", b_), ("QN", qb))
                            rope_proj(lambda k, s_=s_: wq[s_][:, k, 128:192], lambda k, s_=s_: wq[s_][:, k, 192:256], lambda k, q0=q0: cqnT[:, k, q0:q0 + 512],
                                      [("wq", s_), "cqnT"], 4, 512, cosq[:, q0:q0 + 512], sinq[:, q0:q0 + 512], "cosq", QR[0:64, q0:q0 + 512], ("QR", qb), (rt1, rt2))
                        for qb in range(4):
                            q0 = qb * 512
                            for kk in range(66 + 2):
                                if kk < 66:
                                    kt = kk
                                    bs = kt % 3
                                    ps_ = kt % 4
                                    P.op("pe", lambda e, bs=bs, kt=kt, q0=q0: e.matmul(out=mm[bs][:], lhsT=KT[:, kt * 128:(kt + 1) * 128], rhs=QN[:, q0:q0 + 512], start=True, stop=False),
                                         reads=[("KT", kt // 4), ("QN", qb)], writes=[("mm", bs)], inc=False)
                                    P.op("pe", lambda e, bs=bs, kt=kt, q0=q0: e.matmul(out=mm[bs][:], lhsT=kropeT[:, kt * 128:(kt + 1) * 128], rhs=QR[:, q0:q0 + 512], start=False, stop=True),
                                         reads=[("kr", kt // 4), ("QR", qb), "kr_pad", "qr_pad"], writes=[("mm", bs)])
                                    P.op("act", lambda e, bs=bs, ps_=ps_: e.activation(out=PT[ps_][:], in_=mm[bs][:], func=AF.Exp, scale=SCALE), reads=[("mm", bs)], writes=[("PT", ps_)])
                                if kk >= 2:
                                    kt = kk - 2
                                    ps_ = kt % 4
                                    P.op("pe", lambda e, kt=kt, ps_=ps_: e.matmul(out=mm[3][:], lhsT=V[:, kt, :], rhs=PT[ps_][:], start=(kt == 0), stop=(kt == 65)),
                                         reads=[("V", kt // 4), ("PT", ps_)], writes=[("mm", 3)])
                                    P.op("pe", lambda e, kt=kt, ps_=ps_: e.matmul(out=mm[5][:], lhsT=ones_b[:], rhs=PT[ps_][:], start=(kt == 0), stop=(kt == 65)),
                                         reads=[("PT", ps_), "ones_b"], writes=[("mm", 5)])
                            b_ = 5
                            P.op("dve", lambda e, b_=b_: e.reciprocal(out=rden[:], in_=mm[b_][:]), reads=[("mm", b_)], writes=["rden"])
                            ys = (h * 4 + qb) % 2
                            P.op("dve", lambda e, ys=ys: e.tensor_tensor(out=ya[ys][:], in0=mm[3][:], in1=rden[:], op=ALU.mult), reads=[("mm", 3), "rden"], writes=[("ya", ys)])
                            P.dma("sp", st_ymix[:, (8 + h) * NOWN + q0:(8 + h) * NOWN + q0 + 512], ya[ys][:], reads=[("ya", ys)], writes=[("st_ymix", 8 + h, qb)])
                    mm_pool[0] = [0, 1, 2, 3, 4, 5]
                P.barrier()
                if dbg:
                    P.dma("sp", dbgout["d_ymix"], st_ymix, writes=["d_ymix"])

        def phase4():
            with contextlib.ExitStack() as s4:
                wout = sbt(s4, "wout", [128, 16, D], BF16)
                wr = sbt(s4, "wr", [128, 16, NE], BF16)
                G1 = sbt(s4, "G1", [128, D], F32)
                A2 = sbt(s4, "A2", [128, D], F32)
                SH2 = sbt(s4, "SH2", [128, D], F32)
                ym = [sbt(s4, f"ym{i}", [128, 16, 512], BF16) for i in range(2)]
                xt = [sbt(s4, f"xt{i}", [128, D], F32) for i in range(4)]
                tmp = sbt(s4, "tmp", [128, D], F32)
                hb = [sbt(s4, f"hb{i}", [128, D], BF16) for i in range(4)]
                h2g = [sbt(s4, f"h2g{i}", [128, 16, 128], BF16) for i in range(3)]
                affT = sbt(s4, "affT", [16, NOWN], F32)
                sm = sbt(s4, "sm", [128, 16, 4], F32)
                lg = sbt(s4, "lg", [128, 16, NE], F32)
                ex = sbt(s4, "ex", [128, 16, NE], F32)
                print("P4 sbuf bytes remaining", nc.sbuf_bytes_remaining)
                for i in range(4):
                    P.dma("pool", wout[:, i * 4:(i + 1) * 4, :], w_out_d[:, i * 4 * D:(i + 1) * 4 * D].rearrange("p (k n) -> p k n", n=D), writes=["wout"])
                P.dma("pool", wr[:], w_rt_d.rearrange("p (k n) -> p k n", n=NE), writes=["wr"])
                P.dma("sp", G1[:], modrows[0:1, :].partition_broadcast(128), writes=["G1"])
                P.dma("sp", A2[:], modrows[1:2, :].partition_broadcast(128), writes=["A2"])
                P.dma("sp", SH2[:], modrows[2:3, :].partition_broadcast(128), writes=["SH2"])
                pend = []

                def stage_b(p2, t, i, s_):
                    p2()
                    b_ = next_mm()
                    for k in range(16):
                        P.op("pe", lambda e, b_=b_, k=k, t=t: e.matmul(out=mm[b_][:, 0:NE], lhsT=h2g[t % 3][:, k, :], rhs=wr[:, k, :], start=(k == 0), stop=(k == 15)),
                             reads=[("h2g", t % 3), "wr"], writes=[("mm", b_)], inc=(k == 15))
                    P.op("dve", lambda e, b_=b_, t=t: e.tensor_reduce(out=sm[:, t, 0:1], in_=mm[b_][:, 0:NE], axis=AX.X, op=ALU.max), reads=[("mm", b_)], writes=[("sm0", t)])
                    P.op("dve", lambda e, b_=b_, t=t: e.tensor_scalar(out=lg[:, t, :], in0=mm[b_][:, 0:NE], scalar1=sm[:, t, 0:1], scalar2=None, op0=ALU.subtract), reads=[("mm", b_), ("sm0", t)], writes=[("lg", t)])
                    P.op("act", lambda e, t=t: e.activation(out=ex[:, t, :], in_=lg[:, t, :], func=AF.Exp, accum_out=sm[:, t, 1:2]), reads=[("lg", t)], writes=[("ex", t), ("sm1", t)])
                    P.op("dve", lambda e, t=t: e.reciprocal(out=sm[:, t, 2:3], in_=sm[:, t, 1:2]), reads=[("sm1", t)], writes=[("sm2", t)])
                    P.op("dve", lambda e, t=t: e.tensor_scalar(out=afft[:, t, :], in0=ex[:, t, :], scalar1=sm[:, t, 2:3], scalar2=None, op0=ALU.mult), reads=[("ex", t), ("sm2", t)], writes=[("afft", t)])
                    b2 = next_mm()
                    P.op("pe", lambda e, b2=b2, t=t: e.transpose(out=mm[b2][0:NE, 0:128], in_=afft[:, t, :], identity=ident_f[:]), reads=[("afft", t), "ident_f"], writes=[("mm", b2)])
                    P.op("act", lambda e, b2=b2, t=t: e.copy(out=affT[:, t * 128:(t + 1) * 128], in_=mm[b2][0:NE, 0:128]), reads=[("mm", b2)], writes=["affT"])

                for g in range(4):
                    s_ = g % 2
                    P.dma("sp", ym[s_][:], st_ymix.rearrange("p (c n) -> p c n", n=NOWN)[:, :, g * 512:(g + 1) * 512], writes=[("ym", s_)])
                    for i in range(4):
                        t = g * 4 + i
                        xs = t % 4
                        P.dma("sp", xt[xs][:], x_own[t * 128:(t + 1) * 128, :], writes=[("xt", xs)])
                        for db in range(4):
                            b_ = next_mm()
                            for k in range(16):
                                P.op("pe", lambda e, b_=b_, k=k, i=i, db=db, s_=s_: e.matmul(out=mm[b_][:], lhsT=ym[s_][:, k, i * 128:(i + 1) * 128], rhs=wout[:, k, db * 512:(db + 1) * 512], start=(k == 0), stop=(k == 15)),
                                     reads=[("ym", s_), "wout"], writes=[("mm", b_)], inc=(k == 15))
                            P.op("dve", lambda e, b_=b_, db=db: e.tensor_tensor(out=tmp[:, db * 512:(db + 1) * 512], in0=mm[b_][:], in1=G1[:, db * 512:(db + 1) * 512], op=ALU.mult),
                                 reads=[("mm", b_), "G1"], writes=["tmp"])
                        P.op("pool", lambda e, xs=xs: e.tensor_tensor(out=xt[xs][:], in0=tmp[:], in1=xt[xs][:], op=ALU.add), reads=["tmp", ("xt", xs)], writes=[("xt", xs)])
                        P.dma("pool", st_x1[t * 128:(t + 1) * 128, :], xt[xs][:], reads=[("xt", xs)], writes=[("x1", t)])
                        p2 = tok_pipeline(xt[xs], ("xt", xs), hb[xs], ("hb", xs), A2, "A2", SH2, "SH2",
                                          lambda half, t=t: h2g[t % 3][:, half * 8:(half + 1) * 8, :], ("h2g", t % 3), src=None,
                                          post=lambda t=t, xs=xs: P.dma("pool", st_h2[t * 128:(t + 1) * 128, :], hb[xs][:], reads=[("hb", xs)], writes=[("st_h2", t)]),
                                          defer=True)
                        pend.append(lambda p2=p2, t=t, i=i, s_=s_: stage_b(p2, t, i, s_))
                        if len(pend) > 2:
                            pend.pop(0)()
                while pend:
                    pend.pop(0)()
                P.dma("sp", aff_in, affT[:], reads=["affT"], writes=["aff_in"])
                P.coll(lambda e: e.collective_compute("AllGather", ALU.bypass, replica_groups=[[0, 1, 2, 3], [4, 5, 6, 7]], ins=[aff_in], outs=[aff_out]),
                       reads=["aff_in"], writes=["aff_out"])
                P.barrier()
                if dbg:
                    P.dma("sp", dbgout["d_x1"], st_x1, writes=["d_x1"])
                    P.dma("sp", dbgout["d_aff"], aff_out, writes=["d_aff"])
            P.barrier()
            with contextlib.ExitStack() as s5:
                affall = sbt(s5, "affall", [128, NOWN // 2], F32)
                junk = sbt(s5, "junk", [128, NOWN // 2], F32)
                gm64 = sbt(s5, "gm64", [128, 128], F32)
                tt_ = sbt(s5, "tt", [128, 8], F32)
                dg = sbt(s5, "dg", [16, 16], F32)
                thr_rep = sbt(s5, "thr_rep", [128, NE], F32)
                msk = mskp
                ltri = sbt(s5, "ltri", [128, 128], F32)
                wth = sbt(s5, "wth", [128, 16, NE], F32)
                cnts = sbt(s5, "cnts", [128, 16, NE], F32)
                offs = sbt(s5, "offs", [128, 16, NE], F32)
                P.dma("sp", ltri[:], ltri_d, writes=["ltri"])
                lo, tcur, cnt, flag = [tt_[:, i:i + 1] for i in range(4)]
                P.dma("sp", affall[0:64, :], aff_out[:, 0:NOWN // 2], writes=["affall"])
                P.dma("sp", affall[64:128, :], aff_out[:, NOWN // 2:NOWN], writes=["affall"])
                P.dma("sp", gm64[:], gmat_d, writes=["gm64"])
                P.op("dve", lambda e: e.memset(tt_[:], 0.0), writes=["thr"])
                K_ = ["thr"]
                for it in range(THR_ITERS):
                    dl = 2.0 ** (-(it + 1))
                    P.op("dve", lambda e, dl=dl: e.tensor_scalar(out=tcur, in0=lo, scalar1=dl, scalar2=None, op0=ALU.add), reads=K_, writes=K_)
                    P.op("dve", lambda e: e.tensor_scalar(out=junk[:], in0=affall[:], scalar1=tcur, scalar2=None, op0=ALU.is_ge), reads=K_ + ["affall"], writes=["junk"])
                    P.op("dve", lambda e: e.tensor_reduce(out=cnt, in_=junk[:], axis=AX.X, op=ALU.add), reads=["junk"], writes=K_)
                    b_ = next_mm()
                    P.op("pe", lambda e, b_=b_: e.matmul(out=mm[b_][:, 0:1], lhsT=gm64[:], rhs=cnt, start=True, stop=True), reads=K_ + ["gm64"], writes=[("mm", b_)])
                    P.op("dve", lambda e, b_=b_: e.tensor_scalar(out=flag, in0=mm[b_][:, 0:1], scalar1=float(CAP) - 0.5, scalar2=None, op0=ALU.is_ge), reads=[("mm", b_)], writes=K_)
                    P.op("dve", lambda e, dl=dl: e.scalar_tensor_tensor(out=lo, in0=flag, scalar=dl, in1=lo, op0=ALU.mult, op1=ALU.add), reads=K_, writes=K_)
                P.op("dve", lambda e: e.tensor_scalar(out=dg[:], in0=ident_f[0:16, 0:16], scalar1=tt_[0:16, 0:1], scalar2=None, op0=ALU.mult), reads=K_ + ["ident_f"], writes=["dg"])
                b_ = next_mm()
                P.op("pe", lambda e, b_=b_: e.matmul(out=mm[b_][:, 0:NE], lhsT=ones_f[0:16, :], rhs=dg[:], start=True, stop=True), reads=["dg", "ones_f"], writes=[("mm", b_)])
                P.op("act", lambda e, b_=b_: e.copy(out=thr_rep[:], in_=mm[b_][:, 0:NE]), reads=[("mm", b_)], writes=["thr_rep"])
                for t in range(16):
                    P.op("dve", lambda e, t=t: e.tensor_tensor(out=msk[:, t, :], in0=afft[:, t, :], in1=thr_rep[:], op=ALU.is_ge), reads=["thr_rep", ("afft", t)], writes=[("msk", t)])
                    P.op("dve", lambda e, t=t: e.tensor_tensor(out=gmt[:, t, :], in0=msk[:, t, :], in1=afft[:, t, :], op=ALU.mult), reads=[("msk", t), ("afft", t)], writes=[("gmt", t)])
                mk_all = [("msk", t) for t in range(16)]
                m2 = mskp[:].rearrange("p t e -> p (t e)")
                b_ = next_mm()
                P.op("pe", lambda e, b_=b_: e.matmul(out=mm[b_][:, 0:256], lhsT=ltri[:], rhs=m2, start=True, stop=True), reads=mk_all + ["ltri"], writes=[("mm", b_)])
                P.op("act", lambda e, b_=b_: e.copy(out=wth[:].rearrange("p t e -> p (t e)"), in_=mm[b_][:, 0:256]), reads=[("mm", b_)], writes=["wth"])
                b2 = next_mm()
                P.op("pe", lambda e, b2=b2: e.matmul(out=mm[b2][:, 0:256], lhsT=ones_f[:], rhs=m2, start=True, stop=True), reads=mk_all + ["ones_f"], writes=[("mm", b2)])
                P.op("act", lambda e, b2=b2: e.copy(out=cnts[:].rearrange("p t e -> p (t e)"), in_=mm[b2][:, 0:256]), reads=[("mm", b2)], writes=["cnts"])
                P.op("dve", lambda e: e.memset(offs[:, 0, :], 0.0), writes=["offs"])
                for t in range(1, 16):
                    P.op("dve", lambda e, t=t: e.tensor_tensor(out=offs[:, t, :], in0=offs[:, t - 1, :], in1=cnts[:, t - 1, :], op=ALU.add), reads=["offs", "cnts"], writes=["offs"])
                P.op("dve", lambda e: e.tensor_tensor(out=posf[:], in0=wth[:], in1=offs[:], op=ALU.add), reads=["offs", "wth"], writes=["posf"])
                if dbg:
                    P.dma("sp", dbgout["d_thr"], tt_[0:64, 0:2], reads=K_, writes=["d_thr"])
            P.barrier()

        def phase6():
            with contextlib.ExitStack() as s6:
                h2tok = sbt(s6, "h2tok", [128, 16, D], BF16)
                iota = sbt(s6, "iota", [128, CSLOT], F32)
                Pe = sbt(s6, "Pe", [128, 16, CSLOT], BF16)
                PgT = sbt(s6, "PgT", [128, 6, NOWN], BF16)
                xsT = sbt(s6, "xsT", [128, 16, CSLOT], BF16)
                hid = sbt(s6, "hid", [128, NF, CSLOT], BF16)
                ye = sbt(s6, "ye", [128, 6, D], BF16)
                wgu = [sbt(s6, f"wgu{i}", [128, 16, 128], BF16) for i in range(4)]
                wd = [sbt(s6, f"wd{i}", [128, NF, 512], BF16) for i in range(2)]
                sg = [sbt(s6, f"sg{i}", [128, CSLOT], F32) for i in range(2)]
                yst = [sbt(s6, f"yst{i}", [128, 512], F32) for i in range(3)]
                G2 = sbt(s6, "G2", [128, D], F32)
                print("P6 sbuf bytes remaining", nc.sbuf_bytes_remaining)
                P.dma("sp", h2tok[:], st_h2.rearrange("(t p) d -> p t d", p=128), writes=["h2tok"])
                P.dma("sp", iota[:], iota_d, writes=["iota"])
                P.dma("sp", G2[:], modrows[3:4, :].partition_broadcast(128), writes=["G2"])
                wsl = [0]; cnt_ = [0]; ysl = [0]

                def load_w(src):
                    s_ = wsl[0] = (wsl[0] + 1) % 4
                    P.dma("pool", wgu[s_][:], src.rearrange("p (k n) -> p k n", n=128), writes=[("wgu", s_)])
                    return s_

                def evac2(out_ap, in_ap, rk, wkey):
                    c_ = cnt_[0] = cnt_[0] + 1
                    if c_ % 2 == 0:
                        P.op("act", lambda e: e.copy(out=out_ap, in_=in_ap), reads=[rk], writes=[wkey])
                    else:
                        P.op("dve", lambda e: e.tensor_copy(out=out_ap, in_=in_ap), reads=[rk], writes=[wkey])

                for ex_ in range(NE):
                    pi = ex_ % 2
                    for t in range(16):
                        P.op("dve", lambda e, t=t, ex_=ex_: e.tensor_scalar(out=Pe[:, t, :], in0=iota[:], scalar1=posf[:, t, ex_:ex_ + 1], scalar2=mskp[:, t, ex_:ex_ + 1], op0=ALU.is_equal, op1=ALU.mult),
                             reads=["iota"], writes=[("Pe", t)])
                    for dk in range(16):
                        b_ = next_mm()
                        for t in range(16):
                            P.op("pe", lambda e, b_=b_, t=t, dk=dk: e.matmul(out=mm[b_][:, 0:CSLOT], lhsT=h2tok[:, t, dk * 128:(dk + 1) * 128], rhs=Pe[:, t, :], start=(t == 0), stop=(t == 15)),
                                 reads=["h2tok", ("Pe", t)], writes=[("mm", b_)], inc=(t == 15))
                        evac2(xsT[:, dk, :], mm[b_][:, 0:CSLOT], ("mm", b_), ("xsT", dk))
                    for t in range(16):
                        P.op("dve", lambda e, t=t, ex_=ex_: e.tensor_scalar(out=Pe[:, t, :], in0=iota[:], scalar1=posf[:, t, ex_:ex_ + 1], scalar2=gmt[:, t, ex_:ex_ + 1], op0=ALU.is_equal, op1=ALU.mult),
                             reads=["iota"], writes=[("Pe", t)])
                    xk = [("xsT", dk) for dk in range(16)]
                    for f in range(NF):
                        sg_ = load_w(w_g_d[ex_, f]); su_ = load_w(w_u_d[ex_, f])
                        bg_ = next_mm()
                        for k in range(16):
                            P.op("pe", lambda e, bg_=bg_, k=k, sg_=sg_: e.matmul(out=mm[bg_][:, 0:CSLOT], lhsT=wgu[sg_][:, k, :], rhs=xsT[:, k, :], start=(k == 0), stop=(k == 15)),
                                 reads=[("wgu", sg_)] + xk, writes=[("mm", bg_)], inc=(k == 15))
                        bu_ = next_mm()
                        for k in range(16):
                            P.op("pe", lambda e, bu_=bu_, k=k, su_=su_: e.matmul(out=mm[bu_][:, 0:CSLOT], lhsT=wgu[su_][:, k, :], rhs=xsT[:, k, :], start=(k == 0), stop=(k == 15)),
                                 reads=[("wgu", su_)] + xk, writes=[("mm", bu_)], inc=(k == 15))
                        c_ = cnt_[0] = cnt_[0] + 1
                        P.op("act", lambda e, bg_=bg_, c_=c_: e.activation(out=sg[c_ % 2][:], in_=mm[bg_][:, 0:CSLOT], func=AF.Silu), reads=[("mm", bg_)], writes=[("sg", c_ % 2)])
                        P.op("dve", lambda e, bu_=bu_, c_=c_, f=f: e.tensor_tensor(out=hid[:, f, :], in0=sg[c_ % 2][:], in1=mm[bu_][:, 0:CSLOT], op=ALU.mult),
                             reads=[("sg", c_ % 2), ("mm", bu_)], writes=[("hid", f)])
                    for sl in range(3):
                        sg3 = 3 * pi + sl
                        for half in range(2):
                            for k in range(8):
                                t = half * 8 + k
                                P.op("pe", lambda e, half=half, k=k, t=t, sl=sl: e.transpose(out=tp[:, half, k, :], in_=Pe[:, t, sl * 128:(sl + 1) * 128], identity=ident_b[:]),
                                     reads=[("Pe", t), "ident_b"], writes=[("tp", half)], inc=(k == 7))
                            evac2(PgT[:, sg3, half * 1024:(half + 1) * 1024].rearrange("p (k t) -> p k t", t=128), tp[:, half, :, :], ("tp", half), ("PgT", sg3))
                    for db in range(4):
                        ws = (ex_ * 4 + db) % 2
                        P.dma("pool", wd[ws][:], w_d_d[ex_, db].rearrange("p (k n) -> p k n", n=512), writes=[("wd", ws)])
                        for sl in range(3):
                            sg3 = 3 * pi + sl
                            b_ = next_mm()
                            for f in range(NF):
                                P.op("pe", lambda e, b_=b_, f=f, sl=sl, ws=ws: e.matmul(out=mm[b_][:], lhsT=hid[:, f, sl * 128:(sl + 1) * 128], rhs=wd[ws][:, f, :], start=(f == 0), stop=(f == NF - 1)),
                                     reads=[("hid", f), ("wd", ws)], writes=[("mm", b_)], inc=(f == NF - 1))
                            P.op("dve", lambda e, b_=b_, sg3=sg3, db=db: e.tensor_tensor(out=ye[:, sg3, db * 512:(db + 1) * 512], in0=mm[b_][:], in1=G2[:, db * 512:(db + 1) * 512], op=ALU.mult),
                                 reads=[("mm", b_), "G2"], writes=[("ye", sg3, db)])
                    if pi == 0:
                        continue
                    for ti in range(16):
                        for db in range(4):
                            b_ = next_mm()
                            for sl in range(6):
                                P.op("pe", lambda e, b_=b_, sl=sl, ti=ti, db=db: e.matmul(out=mm[b_][:], lhsT=PgT[:, sl, ti * 128:(ti + 1) * 128], rhs=ye[:, sl, db * 512:(db + 1) * 512], start=(sl == 0), stop=(sl == 5)),
                                     reads=[("PgT", sl), ("ye", sl, db)], writes=[("mm", b_)], inc=(sl == 5))
                            ys = ysl[0] = (ysl[0] + 1) % 3
                            evac2(yst[ys][:], mm[b_][:], ("mm", b_), ("yst", ys))
                            P.dma("pool", st_x1[ti * 128:(ti + 1) * 128, db * 512:(db + 1) * 512], yst[ys][:], reads=[("yst", ys)], writes=[("x1", ti, db)], accum_op=ALU.add)
            P.barrier()
            with contextlib.ExitStack() as s7:
                FG = sbt(s7, "FG", [128, D], F32)
                xt = [sbt(s7, f"xt{i}", [128, D], F32) for i in range(4)]
                ot = [sbt(s7, f"ot{i}", [128, D], F32) for i in range(4)]
                jk = sbt(s7, "jk", [128, D], BF16)
                P.dma("sp", FG[:], fg_d.partition_broadcast(128), writes=["FG"])
                for t in range(16):
                    xs = t % 4
                    i = stat_i[0] = (stat_i[0] + 1) % 40
                    sk = f"st{i}"
                    P.dma("sp", xt[xs][:], st_x1[t * 128:(t + 1) * 128, :], writes=[("xt", xs)])
                    ss = stats[:, 4 * i:4 * i + 1]
                    P.op("act", lambda e, xs=xs, ss=ss: e.activation(out=jk[:], in_=xt[xs][:], func=AF.Square, accum_out=ss), reads=[("xt", xs)], writes=["jk", sk])
                    rs, rk = rstd_from_ss(ss, D, sk)
                    P.op("dve", lambda e, xs=xs, rs=rs: e.scalar_tensor_tensor(out=ot[xs][:], in0=xt[xs][:], scalar=rs, in1=FG[:], op0=ALU.mult, op1=ALU.mult), reads=[("xt", xs), rk, "FG"], writes=[("ot", xs)])
                    P.dma("pool", out_d[t * 128:(t + 1) * 128, :], ot[xs][:], reads=[("ot", xs)], writes=[("out", t)])

        with contextlib.ExitStack() as s12:
            A1 = sbt(s12, "A1", [128, D], F32)
            SH1 = sbt(s12, "SH1", [128, D], F32)
            s12.A1, s12.SH1 = A1, SH1
            with contextlib.ExitStack() as s0:
                cpk = sbt(s0, "cpk", [128, 32], F32)
                csl = sbt(s0, "csl", [128, 32], F32)
                rep = sbt(s0, "rep", [128, 32, 128], BF16)
                n1g = sbt(s0, "n1g", [128, D], F32)
                n2g = sbt(s0, "n2g", [128, D], F32)
                wm = [sbt(s0, f"wm{i}", [128, 16, 512], BF16) for i in range(2)]
                bm = [sbt(s0, f"bm{i}", [128, 512], F32) for i in range(2)]
                stg = [sbt(s0, f"stg{i}", [128, 512], F32) for i in range(2)]
                tmpV = sbt(s0, "tmpV", [128, D], F32)
                P.dma("sp", cpk[:], c_pk, writes=["cpk"])
                P.dma("sp", n1g[:], n1g_d.partition_broadcast(128), writes=["n1g"])
                P.dma("sp", n2g[:], n2g_d.partition_broadcast(128), writes=["n2g"])
                P.op("act", lambda e: e.activation(out=csl[:], in_=cpk[:], func=AF.Silu), reads=["cpk"], writes=["csl"])
                for k in range(32):
                    P.op("dve", lambda e, k=k: e.tensor_scalar(out=rep[:, k, :], in0=ones_f[:], scalar1=csl[:, k:k + 1], scalar2=None, op0=ALU.mult),
                         reads=["csl", "ones_f"], writes=["rep"])
                for i in range(8):
                    s_ = i % 2
                    who = 0 if i < 6 else 1
                    P.dma("pool", wm[s_][:], w_mod_d[i].rearrange("p (k n) -> p k n", n=512), writes=[("wm", s_)])
                    P.dma("pool", bm[s_][:], b_mod_d[i:i + 1, :].partition_broadcast(128), writes=[("bm", s_)])
                    b_ = next_mm()
                    for k in range(16):
                        P.op("pe", lambda e, b_=b_, k=k, who=who, s_=s_: e.matmul(out=mm[b_][:], lhsT=rep[:, who * 16 + k, :], rhs=wm[s_][:, k, :], start=(k == 0), stop=(k == 15)),
                             reads=["rep", ("wm", s_)], writes=[("mm", b_)], inc=(k == 15))
                    P.op("dve", lambda e, b_=b_, s_=s_: e.tensor_tensor(out=stg[s_][:], in0=mm[b_][:], in1=bm[s_][:], op=ALU.add), reads=[("mm", b_), ("bm", s_)], writes=[("stg", s_)])
                    P.dma("sp", mod_part[i:i + 1, :], stg[s_][0:1, :], reads=[("stg", s_)], writes=[("mod_part", i)])
                P.coll(lambda e: e.collective_compute("AllGather", ALU.bypass, replica_groups=[[0, 1, 2, 3], [4, 5, 6, 7]], ins=[mod_part], outs=[mod_all]),
                       reads=[("mod_part", i) for i in range(8)], writes=["mod_all"])

                def sec_bcast(dst, sec, key):
                    for q_ in range(4):
                        P.dma("sp", dst[:, q_ * 512:(q_ + 1) * 512], mod_all[q_ * 8 + sec:q_ * 8 + sec + 1, :].partition_broadcast(128), reads=["mod_all"], writes=[key])

                def sec_copy(row, sec):
                    for q_ in range(4):
                        P.dma("sp", modrows[row:row + 1, q_ * 512:(q_ + 1) * 512], mod_all[q_ * 8 + sec:q_ * 8 + sec + 1, :], reads=["mod_all"], writes=[("modrow", row)])

                sec_bcast(SH1, 0, "SH1")
                sec_bcast(tmpV, 1, "tmpV")
                P.op("dve", lambda e: e.scalar_tensor_tensor(out=A1[:], in0=tmpV[:], scalar=1.0, in1=n1g[:], op0=ALU.add, op1=ALU.mult), reads=["tmpV", "n1g"], writes=["A1"])
                sec_copy(0, 2)
                sec_copy(2, 3)
                sec_copy(3, 5)
                sec_copy(5, 6)
                sec_bcast(tmpV, 4, "tmpV")
                P.op("dve", lambda e: e.scalar_tensor_tensor(out=tmpV[:], in0=tmpV[:], scalar=1.0, in1=n2g[:], op0=ALU.add, op1=ALU.mult), reads=["tmpV", "n2g"], writes=["tmpV"])
                P.dma("sp", modrows[1:2, :], tmpV[0:1, :], reads=["tmpV"], writes=[("modrow", 1)])
                sec_bcast(tmpV, 7, "tmpV")
                P.op("dve", lambda e: e.scalar_tensor_tensor(out=tmpV[:], in0=tmpV[:], scalar=1.0, in1=n1g[:], op0=ALU.add, op1=ALU.mult), reads=["tmpV", "n1g"], writes=["tmpV"])
                P.dma("sp", modrows[4:5, :], tmpV[0:1, :], reads=["tmpV"], writes=[("modrow", 4)])
            if dbg:
                P.dma("sp", dbgout["d_modrows"], modrows, reads=[("modrow", r_) for r_ in range(6)], writes=["d_modrows"])
            P.barrier()
            if stop_after >= 1:
                with contextlib.ExitStack() as s1:
                    xnT = sbt(s1, "xnT", [128, 16, NOWN + 128], BF16)
                    with contextlib.ExitStack() as s1a:
                        xt = [sbt(s1a, f"xt{i}", [128, D], F32) for i in range(4)]
                        hb = [sbt(s1a, f"hb{i}", [128, D], BF16) for i in range(4)]
                        for t in range(17):
                            s_ = t % 4
                            if t < 16:
                                src = x_own[t * 128:(t + 1) * 128, :]
                            else:
                                P.op("pool", lambda e, s_=s_: e.memset(xt[s_][:], 0.0), writes=[("xt", s_)])
                                P.dma("sp", xt[s_][0:2, :], x_halo, writes=[("xt", s_)])
                                src = None
                            tok_pipeline(xt[s_], ("xt", s_), hb[s_], ("hb", s_), A1, "A1", SH1, "SH1",
                                         lambda half, t=t: xnT[:, half * 8:(half + 1) * 8, t * 128:(t + 1) * 128], ("xnT", t // 4), src=src)
                    P.barrier()
                    with contextlib.ExitStack() as s1b:
                        wch = [sbt(s1b, f"wch{i}", [128, 16, 128], BF16) for i in range(6)]
                        wkv = sbt(s1b, "wkv", [128, 16, 640], BF16)
                        xin_sb = [sbt(s1b, f"xin_sb{i}", [128, 512], F32) for i in range(2)]
                        Ubuf = sbt(s1b, "Ubuf", [128, NOWN + 2], F32)
                        Uh = sbt(s1b, "Uh", [128, 128], F32)
                        bg_sb = sbt(s1b, "bg_sb", [128, NOWN], F32)
                        Tc = sbt(s1b, "Tc", [128, NOWN], F32)
                        yc = [sbt(s1b, f"yc{i}", [128, NOWN], BF16) for i in range(2)]
                        cw = sbt(s1b, "cw", [128, 24], F32)
                        hmask = sbt(s1b, "hmask", [128, 2], F32)
                        qg = sbt(s1b, "qg", [128, 4], F32)
                        kvg = sbt(s1b, "kvg", [128, 4], F32)
                        raw = sbt(s1b, "raw", [128, 4, 512], F32)
                        sq = sbt(s1b, "sq", [128, 4, 512], BF16)
                        rr = sbt(s1b, "rr", [128, 512], F32)
                        lat = [sbt(s1b, f"lat{i}", [128, 4, 512], BF16) for i in range(2)]
                        rt1 = sbt(s1b, "rt1", [64, 512], F32)
                        rt2 = sbt(s1b, "rt2", [64, 512], F32)
                        cst = sbt(s1b, "cst", [64, 2, 512], F32)
                        krs = [sbt(s1b, f"krs{i}", [64, 512], BF16) for i in range(2)]
                        P.dma("sp", cw[:], conv_d, writes=["cw"])
                        P.dma("sp", hmask[:], hmask_d, writes=["hmask"])
                        P.dma("sp", qg[:], qg_d, writes=["gam"])
                        P.dma("sp", kvg[:], kvg_d, writes=["gam"])
                        for i in range(5):
                            P.dma("pool", wkv[:, :, i * 128:(i + 1) * 128], w_in_d[28 + i].rearrange("p (k n) -> p k n", n=128), writes=["wkv"])
                        wslot = [0]

                        def load_chunk(c):
                            s_ = wslot[0] = (wslot[0] + 1) % 6
                            P.dma("pool", wch[s_][:], w_in_d[c].rearrange("p (k n) -> p k n", n=128), writes=[("wch", s_)])
                            return s_

                        groups = [(0, 512), (512, 512), (1024, 512), (1536, 512), (2048, 128)]
                        for gi in range(4):
                            g0 = gi * 512
                            lt = lat[gi % 2]
                            latent_norm(lambda ci, k: wkv[:, k, ci * 128:(ci + 1) * 128], lambda k, g0=g0: xnT[:, k, g0:g0 + 512],
                                        ["wkv", ("xnT", gi)], kvg, 512,
                                        lambda ci, lt=lt: lt[:, ci, :], ("lat", gi % 2), (raw, sq, rr))
                            for c_ in range(4):
                                P.dma("sp", st_ckvn_l[c_][:, g0:g0 + 512], lt[:, c_, :], reads=[("lat", gi % 2)], writes=[("st_ckvn", gi, c_)])
                            P.dma("sp", cst[:, 0, :], cos_own_d[:, g0:g0 + 512], writes=["cst"])
                            P.dma("sp", cst[:, 1, :], sin_own_d[:, g0:g0 + 512], writes=["cst"])
                            rope_proj(lambda k: wkv[:, k, 512:576], lambda k: wkv[:, k, 576:640], lambda k, g0=g0: xnT[:, k, g0:g0 + 512],
                                      ["wkv", ("xnT", gi)], 16, 512, cst[:, 0, :], cst[:, 1, :], "cst", krs[gi % 2][:], ("krs", gi % 2), (rt1, rt2))
                            P.dma("sp", st_kr[:, g0:g0 + 512], krs[gi % 2][:], reads=[("krs", gi % 2)], writes=[("st_kr", gi)])
                        RG = [[0, 1, 2, 3], [4, 5, 6, 7]]
                        for c_ in range(4):
                            P.coll(lambda e, c_=c_: e.collective_compute("AllGather", ALU.bypass, replica_groups=RG, ins=[st_ckvn_l[c_]], outs=[ag_ck_l[c_]]),
                                   reads=[("st_ckvn", gi, c_) for gi in range(4)], writes=[("ag_ck", c_)])
                        P.coll(lambda e: e.collective_compute("AllGather", ALU.bypass, replica_groups=RG, ins=[st_kr], outs=[ag_kr]),
                               reads=[("st_kr", gi) for gi in range(4)], writes=["ag_kr"])
                        for j in range(8):
                            sx, sb_, sc_ = load_chunk(j), load_chunk(8 + j), load_chunk(16 + j)
                            for gi, (g0, gn) in enumerate(groups):
                                xk = ("xnT", min(gi, 4))
                                ba = next_mm()
                                for k in range(16):
                                    P.op("pe", lambda e, ba=ba, k=k, g0=g0, gn=gn, sx=sx: e.matmul(out=mm[ba][:, 0:gn], lhsT=wch[sx][:, k, :], rhs=xnT[:, k, g0:g0 + gn], start=(k == 0), stop=(k == 15)),
                                         reads=[("wch", sx), xk], writes=[("mm", ba)], inc=(k == 15))
                                xs = xin_sb[gi % 2]
                                P.op("act", lambda e, ba=ba, gn=gn, xs=xs: e.copy(out=xs[:, 0:gn], in_=mm[ba][:, 0:gn]), reads=[("mm", ba)], writes=[("xin_sb", gi % 2)])
                                bc_ = next_mm()
                                for k in range(16):
                                    P.op("pe", lambda e, bc_=bc_, k=k, g0=g0, gn=gn, sc_=sc_: e.matmul(out=mm[bc_][:, 0:gn], lhsT=wch[sc_][:, k, :], rhs=xnT[:, k, g0:g0 + gn], start=(k == 0), stop=(k == 15)),
                                         reads=[("wch", sc_), xk], writes=[("mm", bc_)], inc=(k == 15))
                                if gi < 4:
                                    P.op("dve", lambda e, bc_=bc_, g0=g0, xs=xs: e.tensor_tensor(out=Ubuf[:, 1 + g0:1 + g0 + 512], in0=mm[bc_][:], in1=xs[:], op=ALU.mult),
                                         reads=[("mm", bc_), ("xin_sb", gi % 2)], writes=["Ubuf"])
                                    bb = next_mm()
                                    for k in range(16):
                                        P.op("pe", lambda e, bb=bb, k=k, g0=g0, sb_=sb_: e.matmul(out=mm[bb][:], lhsT=wch[sb_][:, k, :], rhs=xnT[:, k, g0:g0 + 512], start=(k == 0), stop=(k == 15)),
                                             reads=[("wch", sb_), xk], writes=[("mm", bb)], inc=(k == 15))
                                    P.op("act", lambda e, bb=bb, g0=g0: e.copy(out=bg_sb[:, g0:g0 + 512], in_=mm[bb][:]), reads=[("mm", bb)], writes=["bg_sb"])
                                else:
                                    P.op("dve", lambda e, bc_=bc_, xs=xs: e.tensor_tensor(out=Uh[:], in0=mm[bc_][:, 0:128], in1=xs[:, 0:128], op=ALU.mult),
                                         reads=[("mm", bc_), ("xin_sb", gi % 2)], writes=["Uh"])
                            P.op("pool", lambda e: e.tensor_tensor(out=Ubuf[:, 0:1], in0=Uh[:, 0:1], in1=hmask[:, 0:1], op=ALU.mult), reads=["Uh", "hmask"], writes=["Ubuf"])
                            P.op("pool", lambda e: e.tensor_tensor(out=Ubuf[:, NOWN + 1:NOWN + 2], in0=Uh[:, 1:2], in1=hmask[:, 1:2], op=ALU.mult), reads=["Uh", "hmask"], writes=["Ubuf"])
                            P.op("dve", lambda e, j=j: e.tensor_scalar(out=Tc[:], in0=Ubuf[:, 0:NOWN], scalar1=cw[:, 3 * j:3 * j + 1], scalar2=None, op0=ALU.mult), reads=["Ubuf", "cw"], writes=["Tc"])
                            P.op("dve", lambda e, j=j: e.scalar_tensor_tensor(out=Tc[:], in0=Ubuf[:, 1:NOWN + 1], scalar=cw[:, 3 * j + 1:3 * j + 2], in1=Tc[:], op0=ALU.mult, op1=ALU.add), reads=["Ubuf", "cw", "Tc"], writes=["Tc"])
                            P.op("dve", lambda e, j=j: e.scalar_tensor_tensor(out=Tc[:], in0=Ubuf[:, 2:NOWN + 2], scalar=cw[:, 3 * j + 2:3 * j + 3], in1=Tc[:], op0=ALU.mult, op1=ALU.add), reads=["Ubuf", "cw", "Tc"], writes=["Tc"])
                            P.op("dve", lambda e, j=j: e.tensor_tensor(out=yc[j % 2][:], in0=Tc[:], in1=bg_sb[:], op=ALU.mult), reads=["Tc", "bg_sb"], writes=[("yc", j % 2)])
                            P.dma("sp", st_ymix[:, j * NOWN:(j + 1) * NOWN], yc[j % 2][:], reads=[("yc", j % 2)], writes=[("st_ymix", j)])
                        qs = [load_chunk(24 + ci) for ci in range(4)]
                        for gi in range(4):
                            g0 = gi * 512
                            lt = lat[gi % 2]
                            latent_norm(lambda ci, k: wch[qs[ci]][:, k, :], lambda k, g0=g0: xnT[:, k, g0:g0 + 512],
                                        [("wch", s_) for s_ in qs] + [("xnT", gi)], qg, 512,
                                        lambda ci, lt=lt: lt[:, ci, :], ("lat", gi % 2), (raw, sq, rr))
                            P.dma("sp", st_cqn.rearrange("p (c n) -> p c n", n=NOWN)[:, :, g0:g0 + 512], lt[:], reads=[("lat", gi % 2)], writes=[("st_cqn", gi)])
                P.barrier()
            if dbg:
                P.dma("sp", dbgout["d_cqn"], st_cqn, reads=[("st_cqn", g) for g in range(4)], writes=["d_cqn"])
                P.dma("sp", dbgout["d_ymix"], st_ymix, writes=["d_ymix"]) if stop_after < 3 else None
            if stop_after >= 2:
                phase23(s12)
        if stop_after >= 4:
            phase4()
        if stop_after >= 6:
            phase6()
        P.barrier()
        P.run()
    return nc


def _rope_tables(tok):
    tok = np.asarray(tok)
    row = (tok // 64).astype(np.float32)
    col = (tok % 64).astype(np.float32)
    inv = (np.float32(10000.0) ** (-np.arange(16, dtype=np.float32) / np.float32(16))).astype(np.float32)
    ar = row[None, :] * inv[:, None]
    ac = col[None, :] * inv[:, None]
    cos = np.concatenate([np.cos(ar), np.cos(ar), np.cos(ac), np.cos(ac)], 0).astype(np.float32)
    sin = np.concatenate([-np.sin(ar), np.sin(ar), -np.sin(ac), np.sin(ac)], 0).astype(np.float32)
    return np.ascontiguousarray(cos), np.ascontiguousarray(sin)


_SWAP = np.concatenate([np.arange(16, 32), np.arange(0, 16), np.arange(48, 64), np.arange(32, 48)])


def _prep_shared(inp):
    f = lambda a: np.ascontiguousarray(a, dtype=np.float32)
    sh = {}
    w_mod = inp["w_mod"][0]
    w_mod_r = w_mod.reshape(16, 128, 24, 512).transpose(2, 1, 0, 3).reshape(24, 128, 16 * 512)
    b_mod_r = inp["b_mod"][0].reshape(24, 512)
    sh["_w_mod_q"] = [f(w_mod_r[[q + 4 * i for i in range(6)] + [q, q + 4]]) for q in range(4)]
    sh["_b_mod_q"] = [f(b_mod_r[[q + 4 * i for i in range(6)] + [q, q + 4]]) for q in range(4)]
    sh["norm1_g"] = f(inp["norm1_g"][0][None]); sh["norm2_g"] = f(inp["norm2_g"][0][None]); sh["final_g"] = f(inp["final_g"][None])
    w_in = inp["w_in"][0]
    w_ext = np.concatenate([w_in, w_in[:, 4096:][:, _SWAP]], 1)
    sh["w_in_r"] = f(w_ext.reshape(16, 128, 33, 128).transpose(2, 1, 0, 3).reshape(33, 128, 16 * 128))
    sh["conv_r"] = f(inp["conv_w"][0].reshape(3, 8, 128).transpose(2, 1, 0).reshape(128, 24))
    sh["qg_r"] = f(inp["q_norm_g"][0].reshape(4, 128).T); sh["kvg_r"] = f(inp["kv_norm_g"][0].reshape(4, 128).T)
    w_uq = inp["w_uq"][0]
    w_uq_e = np.concatenate([w_uq, w_uq[:, :, 128:][:, :, _SWAP]], 2)
    sh["w_uq_r"] = f(w_uq_e.reshape(4, 128, 8, 256).transpose(2, 1, 0, 3).reshape(8, 128, 4 * 256))
    sh["w_ukv_r"] = f(inp["w_ukv"][0].reshape(4, 128, 8, 256).transpose(2, 1, 0, 3).reshape(8, 128, 4 * 256))
    sh["w_out_r"] = f(inp["w_out"][0].reshape(16, 128, D).transpose(1, 0, 2).reshape(128, 16 * D))
    sh["w_rt_r"] = f(inp["w_router"][0].reshape(16, 128, NE).transpose(1, 0, 2).reshape(128, 16 * NE))
    sh["w_g_r"] = f(inp["w_gate"][0].reshape(NE, 16, 128, NF, 128).transpose(0, 3, 2, 1, 4).reshape(NE, NF, 128, 16 * 128))
    sh["w_u_r"] = f(inp["w_up"][0].reshape(NE, 16, 128, NF, 128).transpose(0, 3, 2, 1, 4).reshape(NE, NF, 128, 16 * 128))
    sh["w_d_r"] = f(inp["w_down"][0].reshape(NE, NF, 128, 4, 512).transpose(0, 3, 2, 1, 4).reshape(NE, 4, 128, NF * 512))
    sh["ident"] = np.eye(128, dtype=np.float32)
    sh["gmat"] = f((np.arange(128)[:, None] % 16) == (np.arange(128)[None, :] % 16))
    sh["ltri"] = f(np.arange(128)[:, None] < np.arange(128)[None, :])
    sh["iota"] = f(np.tile(np.arange(CSLOT, dtype=np.float32)[None], (128, 1)))
    return sh


def _prep_core(inp, r):
    f = lambda a: np.ascontiguousarray(a, dtype=np.float32)
    b, q = r // 4, r % 4
    t0 = q * NOWN
    x = inp["x"][b]
    m = {}
    m["x_own"] = f(x[t0:t0 + NOWN])
    m["x_ctx"] = f(inp["ctx"][b])
    m["x_halo"] = f(np.stack([x[max(t0 - 1, 0)], x[min(t0 + NOWN, 8191)]], 0))
    m["hmask"] = f(np.tile(np.array([[1.0 if q > 0 else 0.0, 1.0 if q < 3 else 0.0]], np.float32), (128, 1)))
    m["c_pk"] = f(np.concatenate([inp["c"][b].reshape(16, 128).T, inp["c_ctx"].reshape(16, 128).T], 1))
    tok_own = np.arange(t0, t0 + NOWN)
    m["cos_own"], m["sin_own"] = _rope_tables(tok_own)
    return m


def kernel(**inputs):
    inp = {k: np.asarray(v) for k, v in inputs.items()}
    nc = build()
    sh = _prep_shared(inp)
    in_maps = []
    for r in range(8):
        m = {k: v for k, v in sh.items() if not k.startswith("_")}
        m["w_mod_q"] = sh["_w_mod_q"][r % 4]
        m["b_mod_q"] = sh["_b_mod_q"][r % 4]
        m.update(_prep_core(inp, r))
        in_maps.append(m)
    res = run_bass_kernel_spmd(nc, in_maps, core_ids=list(range(8)))
    out = np.empty((2, 8192, D), np.float32)
    for r in range(8):
        b, q = r // 4, r % 4
        out[b, q * NOWN:(q + 1) * NOWN] = res.results[r]["out"]
    return out
```

```python
import contextlib
import math
import numpy as np
import concourse.bass as bass
import concourse.mybir as mybir
from concourse.bass_utils import run_bass_kernel_spmd

F32 = mybir.dt.float32
BF16 = mybir.dt.bfloat16
ALU = mybir.AluOpType
AF = mybir.ActivationFunctionType
AX = mybir.AxisListType

D = 2048
NOWN = 2048
NOTH = 6144
NCTX = 256
NKEY = NOWN + NOTH + NCTX
NE = 16
FF = 1408
NF = 11
CAP = 1024
EPS = 1e-6
SCALE = 1.0 / math.sqrt(192.0)
THR_ITERS = 30
CSLOT = 384


class Prog:
    def __init__(self, nc, stack, dma_ring=6):
        self.nc = nc
        names = ["pe", "act", "dve", "pool", "sp"]
        self.sem = {k: stack.enter_context(nc.semaphore("s_" + k)) for k in names}
        self.cnt = {k: 0 for k in names}
        self.lists = {k: [] for k in names}
        self.waited = {k: {} for k in names}
        self.ring = {}
        self.ringn = {}
        for q in ("sp", "pool"):
            self.ring[q] = [stack.enter_context(nc.semaphore(f"d_{q}{i}")) for i in range(dma_ring)]
            self.ringn[q] = 0
        self.csems = [stack.enter_context(nc.semaphore(f"c_{i}")) for i in range(10)]
        self.ncoll = 0
        self.lastw = {}
        self.readers = {}

    def coll(self, fn, reads=(), writes=()):
        reads = list(reads); writes = list(writes)
        sem = self.csems[self.ncoll]; self.ncoll += 1
        waits = self._waits("pool", self._deps(reads, writes))
        self.lists["pool"].append((waits, fn, sem, 1))
        self._record((sem, 1, "coll"), reads, writes)

    def _deps(self, reads, writes):
        deps = []
        for r in reads:
            if r in self.lastw:
                deps.append(self.lastw[r])
        for w in writes:
            if w in self.lastw:
                deps.append(self.lastw[w])
            deps.extend(self.readers.get(w, []))
        return deps

    def _waits(self, e, deps):
        out = []
        for (sem, val, src) in deps:
            if src == e and e == "pe":
                continue
            key = id(sem)
            if self.waited[e].get(key, -1) >= val:
                continue
            self.waited[e][key] = val
            out.append((sem, val))
        return out

    def _record(self, tok, reads, writes):
        for w in writes:
            self.lastw[w] = tok
            self.readers[w] = []
        for r in reads:
            if r not in writes:
                self.readers.setdefault(r, []).append(tok)

    def op(self, e, fn, reads=(), writes=(), inc=True):
        reads = list(reads); writes = list(writes)
        waits = self._waits(e, self._deps(reads, writes))
        sem = self.sem[e]
        if inc:
            self.cnt[e] += 1
            n = self.cnt[e]
        else:
            n = self.cnt[e] + 1
        self.lists[e].append((waits, fn, sem, 1 if inc else 0))
        self._record((sem, n, e), reads, writes)

    def dma(self, q, out, in_, reads=(), writes=(), **kw):
        reads = list(reads); writes = list(writes)
        j = self.ringn[q]; self.ringn[q] += 1
        S = len(self.ring[q])
        sem = self.ring[q][j % S]
        tgt = 16 * (j // S + 1)
        deps = self._deps(reads, writes)
        if j >= S:
            deps.append((sem, tgt - 16, "dma"))
        waits = self._waits(q, deps)
        fn = lambda eng, out=out, in_=in_, kw=kw: eng.dma_start(out=out, in_=in_, **kw)
        self.lists[q].append((waits, fn, sem, 16))
        self._record((sem, tgt, "dma"), reads, writes)

    def dma_fn(self, q, fn, reads=(), writes=()):
        reads = list(reads); writes = list(writes)
        j = self.ringn[q]; self.ringn[q] += 1
        S = len(self.ring[q])
        sem = self.ring[q][j % S]
        tgt = 16 * (j // S + 1)
        deps = self._deps(reads, writes)
        if j >= S:
            deps.append((sem, tgt - 16, "dma"))
        waits = self._waits(q, deps)
        self.lists[q].append((waits, fn, sem, 16))
        self._record((sem, tgt, "dma"), reads, writes)

    def barrier(self):
        deps = []
        for e in self.cnt:
            if self.cnt[e] > 0:
                deps.append((self.sem[e], self.cnt[e], "x"))
        for q in self.ring:
            S = len(self.ring[q]); n = self.ringn[q]
            for i in range(S):
                c = (n - i + S - 1) // S if n > i else 0
                if c > 0:
                    deps.append((self.ring[q][i], 16 * c, "dma"))
        for i in range(self.ncoll):
            deps.append((self.csems[i], 1, "coll"))
        for e in self.lists:
            waits = self._waits(e, [d for d in deps if d[2] != e or e != "pe"])
            self.lists[e].append((waits, None, None, 0))
        self.lastw = {}
        self.readers = {}

    def run(self):
        with self.nc.Block() as block:
            def mk(e):
                def body(eng):
                    for (waits, fn, sem, inc) in self.lists[e]:
                        for (s, v) in waits:
                            eng.wait_ge(s, v)
                        if fn is not None:
                            ins = fn(eng)
                            if inc:
                                ins.then_inc(sem, inc)
                return body
            block.tensor(mk("pe"))
            block.scalar(mk("act"))
            block.vector(mk("dve"))
            block.gpsimd(mk("pool"))
            block.sync(mk("sp"))


def build(dbg=False, stop_after=99):
    nc = bass.Bass("TRN2", target_bir_lowering=False)
    in_names = []

    def dt_in(name, shape):
        in_names.append(name)
        return nc.dram_tensor(name, shape, F32, kind="ExternalInput").ap()
    nc.in_names = in_names
    x_own = dt_in("x_own", [NOWN, D])
    x_ctx = dt_in("x_ctx", [NCTX, D])
    x_halo = dt_in("x_halo", [2, D])
    hmask_d = dt_in("hmask", [128, 2])
    c_pk = dt_in("c_pk", [128, 32])
    cos_own_d = dt_in("cos_own", [64, NOWN]); sin_own_d = dt_in("sin_own", [64, NOWN])
    w_mod_d = dt_in("w_mod_q", [8, 128, 16 * 512])
    b_mod_d = dt_in("b_mod_q", [8, 512])
    n1g_d = dt_in("norm1_g", [1, D]); n2g_d = dt_in("norm2_g", [1, D]); fg_d = dt_in("final_g", [1, D])
    w_in_d = dt_in("w_in_r", [33, 128, 16 * 128])
    conv_d = dt_in("conv_r", [128, 24])
    qg_d = dt_in("qg_r", [128, 4]); kvg_d = dt_in("kvg_r", [128, 4])
    w_uq_d = dt_in("w_uq_r", [8, 128, 4 * 256]); w_ukv_d = dt_in("w_ukv_r", [8, 128, 4 * 256])
    w_out_d = dt_in("w_out_r", [128, 16 * D])
    w_rt_d = dt_in("w_rt_r", [128, 16 * NE])
    if stop_after >= 6:
        w_g_d = dt_in("w_g_r", [NE, NF, 128, 16 * 128]); w_u_d = dt_in("w_u_r", [NE, NF, 128, 16 * 128])
        w_d_d = dt_in("w_d_r", [NE, 4, 128, NF * 512])
    ident_d = dt_in("ident", [128, 128])
    gmat_d = dt_in("gmat", [128, 128])
    ltri_d = dt_in("ltri", [128, 128])
    iota_d = dt_in("iota", [128, CSLOT])
    out_d = nc.dram_tensor("out", [NOWN, D], F32, kind="ExternalOutput").ap()

    modrows = nc.dram_tensor("modrows", [6, D], F32).ap()
    mod_part = nc.dram_tensor("mod_part", [8, 512], F32).ap()
    mod_all = nc.dram_tensor("mod_all", [32, 512], F32).ap()
    st_ymix = nc.dram_tensor("st_ymix", [128, 16 * NOWN], BF16).ap()
    st_cqn = nc.dram_tensor("st_cqn", [128, 4 * NOWN], BF16).ap()
    st_ckvn_l = [nc.dram_tensor(f"st_ckvn{c}", [128, NOWN], BF16).ap() for c in range(4)]
    st_kr = nc.dram_tensor("st_kr", [64, NOWN], BF16).ap()
    st_x1 = nc.dram_tensor("st_x1", [NOWN, D], F32).ap()
    st_h2 = nc.dram_tensor("st_h2", [NOWN, D], BF16).ap()
    aff_in = nc.dram_tensor("aff_in", [16, NOWN], F32).ap()
    aff_out = nc.dram_tensor("aff_out", [64, NOWN], F32).ap()
    ag_ck_l = [nc.dram_tensor(f"ag_ck{c}", [4 * 128, NOWN], BF16).ap() for c in range(4)]
    ag_kr = nc.dram_tensor("ag_kr", [4 * 64, NOWN], BF16).ap()
    dbgout = {}
    if dbg:
        dbgout["d_modrows"] = nc.dram_tensor("d_modrows", [6, D], F32, kind="ExternalOutput").ap()
        dbgout["d_ymix"] = nc.dram_tensor("d_ymix", [128, 16 * NOWN], BF16, kind="ExternalOutput").ap()
        dbgout["d_cqn"] = nc.dram_tensor("d_cqn", [128, 4 * NOWN], BF16, kind="ExternalOutput").ap()
        dbgout["d_ckvn"] = nc.dram_tensor("d_ckvn", [128, 4 * NKEY], BF16, kind="ExternalOutput").ap()
        dbgout["d_kr"] = nc.dram_tensor("d_kr", [64, NKEY], BF16, kind="ExternalOutput").ap()
        dbgout["d_x1"] = nc.dram_tensor("d_x1", [NOWN, D], F32, kind="ExternalOutput").ap()
        dbgout["d_aff"] = nc.dram_tensor("d_aff", [64, NOWN], F32, kind="ExternalOutput").ap()
        dbgout["d_thr"] = nc.dram_tensor("d_thr", [64, 2], F32, kind="ExternalOutput").ap()

    with contextlib.ExitStack() as top:
        P = Prog(nc, top)
        uniq = [0]

        def sbt(st, name, shape, dt):
            uniq[0] += 1
            return st.enter_context(nc.sbuf_tensor(f"sb{uniq[0]}_{name}", shape, dt))
        tp = top.enter_context(nc.psum_tensor("tp", [128, 2, 8, 128], BF16))
        mm = [top.enter_context(nc.psum_tensor(f"mm{i}", [128, 512], F32)) for i in range(6)]
        ident_f = sbt(top, "ident_f", [128, 128], F32)
        ident_b = sbt(top, "ident_b", [128, 128], BF16)
        ones_f = sbt(top, "ones_f", [128, 128], F32)
        ones_b = sbt(top, "ones_b", [128, 128], BF16)
        stats = sbt(top, "stats", [128, 4 * 40], F32)
        afft = sbt(top, "afft", [128, 16, NE], F32)
        gmt = sbt(top, "gmt", [128, 16, NE], F32)
        mskp = sbt(top, "mskp", [128, 16, NE], F32)
        posf = sbt(top, "posf", [128, 16, NE], F32)
        P.dma("sp", ident_f[:], ident_d, writes=["ident_f"])
        P.op("dve", lambda e: e.tensor_copy(out=ident_b[:], in_=ident_f[:]), reads=["ident_f"], writes=["ident_b"])
        P.op("pool", lambda e: e.memset(ones_f[:], 1.0), writes=["ones_f"])
        P.op("pool", lambda e: e.memset(ones_b[:], 1.0), writes=["ones_b"])
        P.op("pool", lambda e: e.memset(stats[:], 0.0), writes=["stats"])
        stat_i = [0]

        def rstd_from_ss(ss_ap, n, key):
            i = stat_i[0]
            c1 = stats[0:ss_ap.shape[0], 4 * i + 1:4 * i + 2]
            c2 = stats[0:ss_ap.shape[0], 4 * i + 2:4 * i + 3]
            c3 = stats[0:ss_ap.shape[0], 4 * i + 3:4 * i + 4]
            P.op("dve", lambda e: e.tensor_scalar(out=c1, in0=ss_ap, scalar1=1.0 / n, scalar2=EPS, op0=ALU.mult, op1=ALU.add), reads=[key], writes=[key + "1"])
            P.op("act", lambda e: e.sqrt(out=c2, in_=c1), reads=[key + "1"], writes=[key + "2"])
            P.op("dve", lambda e: e.reciprocal(out=c3, in_=c2), reads=[key + "2"], writes=[key + "3"])
            return c3, key + "3"

        tile_ctr = [0]

        def tok_pipeline(xt, xt_key, hb, hb_key, A_t, A_key, sh_t, sh_key, dst_fn, dst_key, src=None, post=None, defer=False):
            i = stat_i[0] = (stat_i[0] + 1) % 40
            n = tile_ctr[0] = tile_ctr[0] + 1
            sk = f"st{i}"
            if src is not None:
                P.dma("sp", xt[:], src, writes=[xt_key])
            ss = stats[:, 4 * i:4 * i + 1]
            P.op("act", lambda e: e.activation(out=hb[:], in_=xt[:], func=AF.Square, accum_out=ss), reads=[xt_key], writes=[hb_key, sk])
            rs, rk = rstd_from_ss(ss, D, sk)
            P.op("dve", lambda e: e.scalar_tensor_tensor(out=xt[:], in0=xt[:], scalar=rs, in1=A_t[:], op0=ALU.mult, op1=ALU.mult), reads=[xt_key, rk, A_key], writes=[xt_key])
            P.op("pool", lambda e: e.tensor_tensor(out=hb[:], in0=xt[:], in1=sh_t[:], op=ALU.add), reads=[xt_key, sh_key], writes=[hb_key])
            if post is not None:
                post()

            def part2():
                for half in range(2):
                    for k in range(8):
                        kk = half * 8 + k
                        P.op("pe", lambda e, half=half, k=k, kk=kk: e.transpose(out=tp[:, half, k, :], in_=hb[:, kk * 128:(kk + 1) * 128], identity=ident_b[:]),
                             reads=[hb_key, "ident_b"], writes=[("tp", half)], inc=(k == 7))
                    dst = dst_fn(half)
                    if (n + half) % 2 == 0:
                        P.op("act", lambda e, half=half, dst=dst: e.copy(out=dst, in_=tp[:, half, :, :]), reads=[("tp", half)], writes=[dst_key])
                    else:
                        P.op("dve", lambda e, half=half, dst=dst: e.tensor_copy(out=dst, in_=tp[:, half, :, :]), reads=[("tp", half)], writes=[dst_key])

            if defer:
                return part2
            part2()

        mmrot = [0]
        mm_pool = [[0, 1, 2, 3, 4, 5]]

        def next_mm():
            mmrot[0] = (mmrot[0] + 1) % len(mm_pool[0])
            return mm_pool[0][mmrot[0]]

        def latent_norm(lhs_fn, rhs_fn, rkeys, gamma, N, out_fn, out_key, st):
            raw, sq, rr = st
            for ci in range(4):
                b_ = next_mm()
                for k in range(16):
                    P.op("pe", lambda e, b_=b_, ci=ci, k=k: e.matmul(out=mm[b_][:, 0:N], lhsT=lhs_fn(ci, k), rhs=rhs_fn(k), start=(k == 0), stop=(k == 15)),
                         reads=rkeys, writes=[("mm", b_)], inc=(k == 15))
                P.op("act", lambda e, b_=b_, ci=ci: e.copy(out=raw[:, ci, 0:N], in_=mm[b_][:, 0:N]), reads=[("mm", b_)], writes=[("raw", ci)])
                P.op("act", lambda e, ci=ci: e.activation(out=sq[:, ci, 0:N], in_=raw[:, ci, 0:N], func=AF.Square), reads=[("raw", ci)], writes=[("sq", ci)])
            b2 = next_mm()
            for ci in range(4):
                P.op("pe", lambda e, ci=ci: e.matmul(out=mm[b2][:, 0:N], lhsT=ones_b[:], rhs=sq[:, ci, 0:N], start=(ci == 0), stop=(ci == 3)),
                     reads=[("sq", ci), "ones_b"], writes=[("mm", b2)], inc=(ci == 3))
            P.op("dve", lambda e: e.tensor_scalar(out=rr[:, 0:N], in0=mm[b2][:, 0:N], scalar1=1.0 / 512, scalar2=EPS, op0=ALU.mult, op1=ALU.add), reads=[("mm", b2)], writes=["rr"])
            P.op("act", lambda e: e.sqrt(out=rr[:, 0:N], in_=rr[:, 0:N]), reads=["rr"], writes=["rr"])
            P.op("dve", lambda e: e.reciprocal(out=rr[:, 0:N], in_=rr[:, 0:N]), reads=["rr"], writes=["rr"])
            for ci in range(4):
                eng = "dve"
                P.op(eng, lambda e, ci=ci: e.scalar_tensor_tensor(out=out_fn(ci), in0=raw[:, ci, 0:N], scalar=gamma[:, ci:ci + 1], in1=rr[:, 0:N], op0=ALU.mult, op1=ALU.mult),
                     reads=[("raw", ci), "rr", "gam"], writes=[out_key])

        def rope_proj(lhs1_fn, lhs2_fn, rhs_fn, rkeys, nk, N, cos_t, sin_t, tkey, out_ap, out_key, tmp, do_rope=True):
            b1 = next_mm()
            for k in range(nk):
                P.op("pe", lambda e, k=k: e.matmul(out=mm[b1][0:64, 0:N], lhsT=lhs1_fn(k), rhs=rhs_fn(k), start=(k == 0), stop=(k == nk - 1)),
                     reads=rkeys, writes=[("mm", b1)], inc=(k == nk - 1))
            if not do_rope:
                P.op("act", lambda e: e.copy(out=out_ap, in_=mm[b1][0:64, 0:N]), reads=[("mm", b1)], writes=[out_key])
                return
            b2 = next_mm()
            for k in range(nk):
                P.op("pe", lambda e, k=k: e.matmul(out=mm[b2][0:64, 0:N], lhsT=lhs2_fn(k), rhs=rhs_fn(k), start=(k == 0), stop=(k == nk - 1)),
                     reads=rkeys, writes=[("mm", b2)], inc=(k == nk - 1))
            t1, t2 = tmp
            P.op("dve", lambda e: e.tensor_tensor(out=t1[0:64, 0:N], in0=mm[b1][0:64, 0:N], in1=cos_t, op=ALU.mult), reads=[("mm", b1), tkey], writes=["rt1"])
            P.op("dve", lambda e: e.tensor_tensor(out=t2[0:64, 0:N], in0=mm[b2][0:64, 0:N], in1=sin_t, op=ALU.mult), reads=[("mm", b2), tkey], writes=["rt2"])
            P.op("pool", lambda e: e.tensor_tensor(out=out_ap, in0=t1[0:64, 0:N], in1=t2[0:64, 0:N], op=ALU.add), reads=["rt1", "rt2"], writes=[out_key])


        def phase23(s12):
            A1, SH1 = s12.A1, s12.SH1
            with contextlib.ExitStack() as s23:
                ckvnT = sbt(s23, "ckvnT", [128, 4, NKEY], BF16)
                kropeT = sbt(s23, "kropeT", [128, NKEY], BF16)
                P.op("pool", lambda e: e.memset(kropeT[64:128, :], 0.0), writes=["kr_pad"])
                with contextlib.ExitStack() as s2:
                    xt = [sbt(s2, f"xt{i}", [128, D], F32) for i in range(2)]
                    hb = [sbt(s2, f"hb{i}", [128, D], BF16) for i in range(2)]
                    xg = [sbt(s2, f"xg{i}", [128, 16, 512], BF16) for i in range(2)]
                    wkv = sbt(s2, "wkv", [128, 16, 640], BF16)
                    kvg = sbt(s2, "kvg", [128, 4], F32)
                    raw = sbt(s2, "raw", [128, 4, 512], F32)
                    sq = sbt(s2, "sq", [128, 4, 512], BF16)
                    rr = sbt(s2, "rr", [128, 512], F32)
                    rt1 = sbt(s2, "rt1", [64, 512], F32)
                    rt2 = sbt(s2, "rt2", [64, 512], F32)
                    cst = sbt(s2, "cst", [64, 2, 512], F32)
                    P.dma("sp", kvg[:], kvg_d, writes=["gam"])
                    for i in range(5):
                        P.dma("pool", wkv[:, :, i * 128:(i + 1) * 128], w_in_d[28 + i].rearrange("p (k n) -> p k n", n=128), writes=["wkv"])
                    for g in range(12, 13):
                        s_ = g % 2
                        n = 512 if g < 12 else NCTX
                        koff = NOWN + g * 512
                        blk = 4 + g
                        if g == 12:
                            P.dma("sp", A1[:], modrows[4:5, :].partition_broadcast(128), writes=["A1"])
                            P.dma("sp", SH1[:], modrows[5:6, :].partition_broadcast(128), writes=["SH1"])
                        for t in range(n // 128):
                            tt = g * 4 + t
                            xs = tt % 2
                            src = x_ctx[t * 128:(t + 1) * 128, :]
                            tok_pipeline(xt[xs], ("xt", xs), hb[xs], ("hb", xs), A1, "A1", SH1, "SH1",
                                         lambda half, t=t, s_=s_: xg[s_][:, half * 8:(half + 1) * 8, t * 128:(t + 1) * 128], ("xg", s_), src=src)
                        latent_norm(lambda ci, k: wkv[:, k, ci * 128:(ci + 1) * 128], lambda k, s_=s_, n=n: xg[s_][:, k, 0:n],
                                    ["wkv", ("xg", s_)], kvg, n,
                                    lambda ci, koff=koff, n=n: ckvnT[:, ci, koff:koff + n], ("ck", blk), (raw, sq, rr))
                        rope_proj(lambda k: wkv[:, k, 512:576], lambda k: wkv[:, k, 576:640], lambda k, s_=s_, n=n: xg[s_][:, k, 0:n],
                                  ["wkv", ("xg", s_)], 16, n, cst[:, 0, 0:n], cst[:, 1, 0:n], "cst", kropeT[0:64, koff:koff + n], ("kr", blk), (rt1, rt2), do_rope=(g < 12))
                for r_ in range(4):
                    for c_ in range(4):
                        P.dma("sp", ckvnT[:, c_, r_ * NOWN:(r_ + 1) * NOWN], ag_ck_l[c_][r_ * 128:(r_ + 1) * 128, :],
                              reads=[("ag_ck", c_)], writes=[("ck", 4 * r_ + b_) for b_ in range(4)])
                    P.dma("sp", kropeT[0:64, r_ * NOWN:(r_ + 1) * NOWN], ag_kr[r_ * 64:(r_ + 1) * 64, :], reads=["ag_kr"], writes=[("kr", 4 * r_ + b_) for b_ in range(4)])
                P.barrier()
                if dbg:
                    P.dma("sp", dbgout["d_ckvn"].rearrange("p (c n) -> p c n", n=NKEY), ckvnT[:], writes=["d_ckvn"])
                    P.dma("sp", dbgout["d_kr"], kropeT[0:64, :], writes=["d_kr"])
                if stop_after < 3:
                    return
                with contextlib.ExitStack() as s3:
                    cqnT = sbt(s3, "cqnT", [128, 4, NOWN], BF16)
                    cosq = sbt(s3, "cosq", [64, NOWN], F32)
                    sinq = sbt(s3, "sinq", [64, NOWN], F32)
                    wq = [sbt(s3, f"wq{i}", [128, 4, 256], BF16) for i in range(2)]
                    wk = [sbt(s3, f"wk{i}", [128, 4, 256], BF16) for i in range(2)]
                    KT = sbt(s3, "KT", [128, NKEY], BF16)
                    V = sbt(s3, "V", [128, 66, 128], BF16)
                    QN = sbt(s3, "QN", [128, NOWN], BF16)
                    QR = sbt(s3, "QR", [128, NOWN], BF16)
                    P.op("pool", lambda e: e.memset(QR[64:128, :], 0.0), writes=["qr_pad"])
                    PT = [sbt(s3, f"PT{i}", [128, 512], BF16) for i in range(4)]
                    denA = sbt(s3, "denA", [128, 512], F32)
                    denB = sbt(s3, "denB", [128, 512], F32)
                    rden = sbt(s3, "rden", [128, 512], F32)
                    ya = [sbt(s3, f"ya{i}", [128, 512], BF16) for i in range(2)]
                    rt1 = sbt(s3, "rt1", [64, 512], F32)
                    rt2 = sbt(s3, "rt2", [64, 512], F32)
                    P.dma("sp", cqnT[:], st_cqn.rearrange("p (c n) -> p c n", n=NOWN), writes=["cqnT"])
                    P.dma("sp", cosq[:], cos_own_d, writes=["cosq"])
                    P.dma("sp", sinq[:], sin_own_d, writes=["cosq"])
                    mm_pool[0] = [4, 5]
                    ev = [0]

                    def evac(out_ap, in_ap, rk, wkey):
                        ev[0] += 1
                        if ev[0] % 2 == 0:
                            P.op("act", lambda e: e.copy(out=out_ap, in_=in_ap), reads=[rk], writes=[wkey])
                        else:
                            P.op("dve", lambda e: e.tensor_copy(out=out_ap, in_=in_ap), reads=[rk], writes=[wkey])

                    for h in range(8):
                        s_ = h % 2
                        P.dma("pool", wq[s_][:], w_uq_d[h].rearrange("p (k n) -> p k n", n=256), writes=[("wq", s_)])
                        P.dma("pool", wk[s_][:], w_ukv_d[h].rearrange("p (k n) -> p k n", n=256), writes=[("wk", s_)])
                        for nb in range(17):
                            n = 512 if nb < 16 else 256
                            b_ = next_mm()
                            for k in range(4):
                                P.op("pe", lambda e, b_=b_, k=k, nb=nb, n=n, s_=s_: e.matmul(out=mm[b_][:, 0:n], lhsT=wk[s_][:, k, 0:128], rhs=ckvnT[:, k, nb * 512:nb * 512 + n], start=(k == 0), stop=(k == 3)),
                                     reads=[("wk", s_), ("ck", nb)], writes=[("mm", b_)], inc=(k == 3))
                            evac(KT[:, nb * 512:nb * 512 + n], mm[b_][:, 0:n], ("mm", b_), ("KT", nb))
                        for vb in range(17):
                            nt = 4 if vb < 16 else 2
                            b_ = next_mm()
                            for i in range(nt):
                                kt = vb * 4 + i
                                for k in range(4):
                                    P.op("pe", lambda e, b_=b_, k=k, i=i, kt=kt, s_=s_: e.matmul(out=mm[b_][:, i * 128:(i + 1) * 128], lhsT=ckvnT[:, k, kt * 128:(kt + 1) * 128], rhs=wk[s_][:, k, 128:256], start=(k == 0), stop=(k == 3)),
                                         reads=[("wk", s_), ("ck", vb)], writes=[("mm", b_)], inc=(k == 3 and i == nt - 1))
                            evac(V[:, vb * 4:vb * 4 + nt, :], mm[b_][:, 0:nt * 128].rearrange("p (t d) -> p t d", d=128), ("mm", b_), ("V", vb))
                        for qb in range(4):
                            q0 = qb * 512
                            b_ = next_mm()
                            for k in range(4):
                                P.op("pe", lambda e, b_=b_, k=k, q0=q0, s_=s_: e.matmul(out=mm[b_][:], lhsT=wq[s_][:, k, 0:128], rhs=cqnT[:, k, q0:q0 + 512], start=(k == 0), stop=(k == 3)),
                                     reads=[("wq", s_), "cqnT"], writes=[("mm", b_)], inc=(k == 3))
                            evac(QN[:, q0:q0 + 512], mm[b_][:], ("mm", b_), ("QN", qb))
                            rope_proj(lambda k, s_=s_: wq[s_][:, k, 128:192], lambda k, s_=s_: wq[s_][:, k, 192:256], lambda k, q0=q0: cqnT[:, k, q0:q0 + 512],
                                      [("wq", s_), "cqnT"], 4, 512, cosq[:, q0:q0 + 512], sinq[:, q0:q0 + 512], "cosq", QR[0:64, q0:q0 + 512], ("QR", qb), (rt1, rt2))
                        for qb in range(4):
                            q0 = qb * 512
                            for kk in range(66 + 2):
                                if kk < 66:
                                    kt = kk
                                    bs = kt % 3
                                    ps_ = kt % 4
                                    P.op("pe", lambda e, bs=bs, kt=kt, q0=q0: e.matmul(out=mm[bs][:], lhsT=KT[:, kt * 128:(kt + 1) * 128], rhs=QN[:, q0:q0 + 512], start=True, stop=False),
                                         reads=[("KT", kt // 4), ("QN", qb)], writes=[("mm", bs)], inc=False)
                                    P.op("pe", lambda e, bs=bs, kt=kt, q0=q0: e.matmul(out=mm[bs][:], lhsT=kropeT[:, kt * 128:(kt + 1) * 128], rhs=QR[:, q0:q0 + 512], start=False, stop=True),
                                         reads=[("kr", kt // 4), ("QR", qb), "kr_pad", "qr_pad"], writes=[("mm", bs)])
                                    P.op("act", lambda e, bs=bs, ps_=ps_: e.activation(out=PT[ps_][:], in_=mm[bs][:], func=AF.Exp, scale=SCALE), reads=[("mm", bs)], writes=[("PT", ps_)])
                                if kk >= 2:
                                    kt = kk - 2
                                    ps_ = kt % 4
                                    P.op("pe", lambda e, kt=kt, ps_=ps_: e.matmul(out=mm[3][:], lhsT=V[:, kt, :], rhs=PT[ps_][:], start=(kt == 0), stop=(kt == 65)),
                                         reads=[("V", kt // 4), ("PT", ps_)], writes=[("mm", 3)])
                                    P.op("pe", lambda e, kt=kt, ps_=ps_: e.matmul(out=mm[5][:], lhsT=ones_b[:], rhs=PT[ps_][:], start=(kt == 0), stop=(kt == 65)),
                                         reads=[("PT", ps_), "ones_b"], writes=[("mm", 5)])
                            b_ = 5
                            P.op("dve", lambda e, b_=b_: e.reciprocal(out=rden[:], in_=mm[b_][:]), reads=[("mm", b_)], writes=["rden"])
                            ys = (h * 4 + qb) % 2
                            P.op("dve", lambda e, ys=ys: e.tensor_tensor(out=ya[ys][:], in0=mm[3][:], in1=rden[:], op=ALU.mult), reads=[("mm", 3), "rden"], writes=[("ya", ys)])
                            P.dma("sp", st_ymix[:, (8 + h) * NOWN + q0:(8 + h) * NOWN + q0 + 512], ya[ys][:], reads=[("ya", ys)], writes=[("st_ymix", 8 + h, qb)])
                    mm_pool[0] = [0, 1, 2, 3, 4, 5]
                P.barrier()
                if dbg:
                    P.dma("sp", dbgout["d_ymix"], st_ymix, writes=["d_ymix"])

        def phase4():
            with contextlib.ExitStack() as s4:
                wout = sbt(s4, "wout", [128, 16, D], BF16)
                wr = sbt(s4, "wr", [128, 16, NE], BF16)
                G1 = sbt(s4, "G1", [128, D], F32)
                A2 = sbt(s4, "A2", [128, D], F32)
                SH2 = sbt(s4, "SH2", [128, D], F32)
                ym = [sbt(s4, f"ym{i}", [128, 16, 512], BF16) for i in range(2)]
                xt = [sbt(s4, f"xt{i}", [128, D], F32) for i in range(4)]
                tmp = sbt(s4, "tmp", [128, D], F32)
                hb = [sbt(s4, f"hb{i}", [128, D], BF16) for i in range(4)]
                h2g = [sbt(s4, f"h2g{i}", [128, 16, 128], BF16) for i in range(3)]
                affT = sbt(s4, "affT", [16, NOWN], F32)
                sm = sbt(s4, "sm", [128, 16, 4], F32)
                lg = sbt(s4, "lg", [128, 16, NE], F32)
                ex = sbt(s4, "ex", [128, 16, NE], F32)
                print("P4 sbuf bytes remaining", nc.sbuf_bytes_remaining)
                for i in range(4):
                    P.dma("pool", wout[:, i * 4:(i + 1) * 4, :], w_out_d[:, i * 4 * D:(i + 1) * 4 * D].rearrange("p (k n) -> p k n", n=D), writes=["wout"])
                P.dma("pool", wr[:], w_rt_d.rearrange("p (k n) -> p k n", n=NE), writes=["wr"])
                P.dma("sp", G1[:], modrows[0:1, :].partition_broadcast(128), writes=["G1"])
                P.dma("sp", A2[:], modrows[1:2, :].partition_broadcast(128), writes=["A2"])
                P.dma("sp", SH2[:], modrows[2:3, :].partition_broadcast(128), writes=["SH2"])
                pend = []

                def stage_b(p2, t, i, s_):
                    p2()
                    b_ = next_mm()
                    for k in range(16):
                        P.op("pe", lambda e, b_=b_, k=k, t=t: e.matmul(out=mm[b_][:, 0:NE], lhsT=h2g[t % 3][:, k, :], rhs=wr[:, k, :], start=(k == 0), stop=(k == 15)),
                             reads=[("h2g", t % 3), "wr"], writes=[("mm", b_)], inc=(k == 15))
                    P.op("dve", lambda e, b_=b_, t=t: e.tensor_reduce(out=sm[:, t, 0:1], in_=mm[b_][:, 0:NE], axis=AX.X, op=ALU.max), reads=[("mm", b_)], writes=[("sm0", t)])
                    P.op("dve", lambda e, b_=b_, t=t: e.tensor_scalar(out=lg[:, t, :], in0=mm[b_][:, 0:NE], scalar1=sm[:, t, 0:1], scalar2=None, op0=ALU.subtract), reads=[("mm", b_), ("sm0", t)], writes=[("lg", t)])
                    P.op("act", lambda e, t=t: e.activation(out=ex[:, t, :], in_=lg[:, t, :], func=AF.Exp, accum_out=sm[:, t, 1:2]), reads=[("lg", t)], writes=[("ex", t), ("sm1", t)])
                    P.op("dve", lambda e, t=t: e.reciprocal(out=sm[:, t, 2:3], in_=sm[:, t, 1:2]), reads=[("sm1", t)], writes=[("sm2", t)])
                    P.op("dve", lambda e, t=t: e.tensor_scalar(out=afft[:, t, :], in0=ex[:, t, :], scalar1=sm[:, t, 2:3], scalar2=None, op0=ALU.mult), reads=[("ex", t), ("sm2", t)], writes=[("afft", t)])
                    b2 = next_mm()
                    P.op("pe", lambda e, b2=b2, t=t: e.transpose(out=mm[b2][0:NE, 0:128], in_=afft[:, t, :], identity=ident_f[:]), reads=[("afft", t), "ident_f"], writes=[("mm", b2)])
                    P.op("act", lambda e, b2=b2, t=t: e.copy(out=affT[:, t * 128:(t + 1) * 128], in_=mm[b2][0:NE, 0:128]), reads=[("mm", b2)], writes=["affT"])

                for g in range(4):
                    s_ = g % 2
                    P.dma("sp", ym[s_][:], st_ymix.rearrange("p (c n) -> p c n", n=NOWN)[:, :, g * 512:(g + 1) * 512], writes=[("ym", s_)])
                    for i in range(4):
                        t = g * 4 + i
                        xs = t % 4
                        P.dma("sp", xt[xs][:], x_own[t * 128:(t + 1) * 128, :], writes=[("xt", xs)])
                        for db in range(4):
                            b_ = next_mm()
                            for k in range(16):
                                P.op("pe", lambda e, b_=b_, k=k, i=i, db=db, s_=s_: e.matmul(out=mm[b_][:], lhsT=ym[s_][:, k, i * 128:(i + 1) * 128], rhs=wout[:, k, db * 512:(db + 1) * 512], start=(k == 0), stop=(k == 15)),
                                     reads=[("ym", s_), "wout"], writes=[("mm", b_)], inc=(k == 15))
                            P.op("dve", lambda e, b_=b_, db=db: e.tensor_tensor(out=tmp[:, db * 512:(db + 1) * 512], in0=mm[b_][:], in1=G1[:, db * 512:(db + 1) * 512], op=ALU.mult),
                                 reads=[("mm", b_), "G1"], writes=["tmp"])
                        P.op("pool", lambda e, xs=xs: e.tensor_tensor(out=xt[xs][:], in0=tmp[:], in1=xt[xs][:], op=ALU.add), reads=["tmp", ("xt", xs)], writes=[("xt", xs)])
                        P.dma("pool", st_x1[t * 128:(t + 1) * 128, :], xt[xs][:], reads=[("xt", xs)], writes=[("x1", t)])
                        p2 = tok_pipeline(xt[xs], ("xt", xs), hb[xs], ("hb", xs), A2, "A2", SH2, "SH2",
                                          lambda half, t=t: h2g[t % 3][:, half * 8:(half + 1) * 8, :], ("h2g", t % 3), src=None,
                                          post=lambda t=t, xs=xs: P.dma("pool", st_h2[t * 128:(t + 1) * 128, :], hb[xs][:], reads=[("hb", xs)], writes=[("st_h2", t)]),
                                          defer=True)
                        pend.append(lambda p2=p2, t=t, i=i, s_=s_: stage_b(p2, t, i, s_))
                        if len(pend) > 2:
                            pend.pop(0)()
                while pend:
                    pend.pop(0)()
                P.dma("sp", aff_in, affT[:], reads=["affT"], writes=["aff_in"])
                P.coll(lambda e: e.collective_compute("AllGather", ALU.bypass, replica_groups=[[0, 1, 2, 3], [4, 5, 6, 7]], ins=[aff_in], outs=[aff_out]),
                       reads=["aff_in"], writes=["aff_out"])
                P.barrier()
                if dbg:
                    P.dma("sp", dbgout["d_x1"], st_x1, writes=["d_x1"])
                    P.dma("sp", dbgout["d_aff"], aff_out, writes=["d_aff"])
            P.barrier()
            with contextlib.ExitStack() as s5:
                affall = sbt(s5, "affall", [128, NOWN // 2], F32)
                junk = sbt(s5, "junk", [128, NOWN // 2], F32)
                gm64 = sbt(s5, "gm64", [128, 128], F32)
                tt_ = sbt(s5, "tt", [128, 8], F32)
                dg = sbt(s5, "dg", [16, 16], F32)
                thr_rep = sbt(s5, "thr_rep", [128, NE], F32)
                msk = mskp
                ltri = sbt(s5, "ltri", [128, 128], F32)
                wth = sbt(s5, "wth", [128, 16, NE], F32)
                cnts = sbt(s5, "cnts", [128, 16, NE], F32)
                offs = sbt(s5, "offs", [128, 16, NE], F32)
                P.dma("sp", ltri[:], ltri_d, writes=["ltri"])
                lo, tcur, cnt, flag = [tt_[:, i:i + 1] for i in range(4)]
                P.dma("sp", affall[0:64, :], aff_out[:, 0:NOWN // 2], writes=["affall"])
                P.dma("sp", affall[64:128, :], aff_out[:, NOWN // 2:NOWN], writes=["affall"])
                P.dma("sp", gm64[:], gmat_d, writes=["gm64"])
                P.op("dve", lambda e: e.memset(tt_[:], 0.0), writes=["thr"])
                K_ = ["thr"]
                for it in range(THR_ITERS):
                    dl = 2.0 ** (-(it + 1))
                    P.op("dve", lambda e, dl=dl: e.tensor_scalar(out=tcur, in0=lo, scalar1=dl, scalar2=None, op0=ALU.add), reads=K_, writes=K_)
                    P.op("dve", lambda e: e.tensor_scalar(out=junk[:], in0=affall[:], scalar1=tcur, scalar2=None, op0=ALU.is_ge), reads=K_ + ["affall"], writes=["junk"])
                    P.op("dve", lambda e: e.tensor_reduce(out=cnt, in_=junk[:], axis=AX.X, op=ALU.add), reads=["junk"], writes=K_)
                    b_ = next_mm()
                    P.op("pe", lambda e, b_=b_: e.matmul(out=mm[b_][:, 0:1], lhsT=gm64[:], rhs=cnt, start=True, stop=True), reads=K_ + ["gm64"], writes=[("mm", b_)])
                    P.op("dve", lambda e, b_=b_: e.tensor_scalar(out=flag, in0=mm[b_][:, 0:1], scalar1=float(CAP) - 0.5, scalar2=None, op0=ALU.is_ge), reads=[("mm", b_)], writes=K_)
                    P.op("dve", lambda e, dl=dl: e.scalar_tensor_tensor(out=lo, in0=flag, scalar=dl, in1=lo, op0=ALU.mult, op1=ALU.add), reads=K_, writes=K_)
                P.op("dve", lambda e: e.tensor_scalar(out=dg[:], in0=ident_f[0:16, 0:16], scalar1=tt_[0:16, 0:1], scalar2=None, op0=ALU.mult), reads=K_ + ["ident_f"], writes=["dg"])
                b_ = next_mm()
                P.op("pe", lambda e, b_=b_: e.matmul(out=mm[b_][:, 0:NE], lhsT=ones_f[0:16, :], rhs=dg[:], start=True, stop=True), reads=["dg", "ones_f"], writes=[("mm", b_)])
                P.op("act", lambda e, b_=b_: e.copy(out=thr_rep[:], in_=mm[b_][:, 0:NE]), reads=[("mm", b_)], writes=["thr_rep"])
                for t in range(16):
                    P.op("dve", lambda e, t=t: e.tensor_tensor(out=msk[:, t, :], in0=afft[:, t, :], in1=thr_rep[:], op=ALU.is_ge), reads=["thr_rep", ("afft", t)], writes=[("msk", t)])
                    P.op("dve", lambda e, t=t: e.tensor_tensor(out=gmt[:, t, :], in0=msk[:, t, :], in1=afft[:, t, :], op=ALU.mult), reads=[("msk", t), ("afft", t)], writes=[("gmt", t)])
                mk_all = [("msk", t) for t in range(16)]
                m2 = mskp[:].rearrange("p t e -> p (t e)")
                b_ = next_mm()
                P.op("pe", lambda e, b_=b_: e.matmul(out=mm[b_][:, 0:256], lhsT=ltri[:], rhs=m2, start=True, stop=True), reads=mk_all + ["ltri"], writes=[("mm", b_)])
                P.op("act", lambda e, b_=b_: e.copy(out=wth[:].rearrange("p t e -> p (t e)"), in_=mm[b_][:, 0:256]), reads=[("mm", b_)], writes=["wth"])
                b2 = next_mm()
                P.op("pe", lambda e, b2=b2: e.matmul(out=mm[b2][:, 0:256], lhsT=ones_f[:], rhs=m2, start=True, stop=True), reads=mk_all + ["ones_f"], writes=[("mm", b2)])
                P.op("act", lambda e, b2=b2: e.copy(out=cnts[:].rearrange("p t e -> p (t e)"), in_=mm[b2][:, 0:256]), reads=[("mm", b2)], writes=["cnts"])
                P.op("dve", lambda e: e.memset(offs[:, 0, :], 0.0), writes=["offs"])
                for t in range(1, 16):
                    P.op("dve", lambda e, t=t: e.tensor_tensor(out=offs[:, t, :], in0=offs[:, t - 1, :], in1=cnts[:, t - 1, :], op=ALU.add), reads=["offs", "cnts"], writes=["offs"])
                P.op("dve", lambda e: e.tensor_tensor(out=posf[:], in0=wth[:], in1=offs[:], op=ALU.add), reads=["offs", "wth"], writes=["posf"])
                if dbg:
                    P.dma("sp", dbgout["d_thr"], tt_[0:64, 0:2], reads=K_, writes=["d_thr"])
            P.barrier()

        def phase6():
            with contextlib.ExitStack() as s6:
                h2tok = sbt(s6, "h2tok", [128, 16, D], BF16)
                iota = sbt(s6, "iota", [128, CSLOT], F32)
                Pe = sbt(s6, "Pe", [128, 16, CSLOT], BF16)
                PgT = sbt(s6, "PgT", [128, 6, NOWN], BF16)
                xsT = sbt(s6, "xsT", [128, 16, CSLOT], BF16)
                hid = sbt(s6, "hid", [128, NF, CSLOT], BF16)
                ye = sbt(s6, "ye", [128, 6, D], BF16)
                wgu = [sbt(s6, f"wgu{i}", [128, 16, 128], BF16) for i in range(4)]
                wd = [sbt(s6, f"wd{i}", [128, NF, 512], BF16) for i in range(2)]
                sg = [sbt(s6, f"sg{i}", [128, CSLOT], F32) for i in range(2)]
                yst = [sbt(s6, f"yst{i}", [128, 512], F32) for i in range(3)]
                G2 = sbt(s6, "G2", [128, D], F32)
                print("P6 sbuf bytes remaining", nc.sbuf_bytes_remaining)
                P.dma("sp", h2tok[:], st_h2.rearrange("(t p) d -> p t d", p=128), writes=["h2tok"])
                P.dma("sp", iota[:], iota_d, writes=["iota"])
                P.dma("sp", G2[:], modrows[3:4, :].partition_broadcast(128), writes=["G2"])
                wsl = [0]; cnt_ = [0]; ysl = [0]

                def load_w(src):
                    s_ = wsl[0] = (wsl[0] + 1) % 4
                    P.dma("pool", wgu[s_][:], src.rearrange("p (k n) -> p k n", n=128), writes=[("wgu", s_)])
                    return s_

                def evac2(out_ap, in_ap, rk, wkey):
                    c_ = cnt_[0] = cnt_[0] + 1
                    if c_ % 2 == 0:
                        P.op("act", lambda e: e.copy(out=out_ap, in_=in_ap), reads=[rk], writes=[wkey])
                    else:
                        P.op("dve", lambda e: e.tensor_copy(out=out_ap, in_=in_ap), reads=[rk], writes=[wkey])

                for ex_ in range(NE):
                    pi = ex_ % 2
                    for t in range(16):
                        P.op("dve", lambda e, t=t, ex_=ex_: e.tensor_scalar(out=Pe[:, t, :], in0=iota[:], scalar1=posf[:, t, ex_:ex_ + 1], scalar2=mskp[:, t, ex_:ex_ + 1], op0=ALU.is_equal, op1=ALU.mult),
                             reads=["iota"], writes=[("Pe", t)])
                    for dk in range(16):
                        b_ = next_mm()
                        for t in range(16):
                            P.op("pe", lambda e, b_=b_, t=t, dk=dk: e.matmul(out=mm[b_][:, 0:CSLOT], lhsT=h2tok[:, t, dk * 128:(dk + 1) * 128], rhs=Pe[:, t, :], start=(t == 0), stop=(t == 15)),
                                 reads=["h2tok", ("Pe", t)], writes=[("mm", b_)], inc=(t == 15))
                        evac2(xsT[:, dk, :], mm[b_][:, 0:CSLOT], ("mm", b_), ("xsT", dk))
                    for t in range(16):
                        P.op("dve", lambda e, t=t, ex_=ex_: e.tensor_scalar(out=Pe[:, t, :], in0=iota[:], scalar1=posf[:, t, ex_:ex_ + 1], scalar2=gmt[:, t, ex_:ex_ + 1], op0=ALU.is_equal, op1=ALU.mult),
                             reads=["iota"], writes=[("Pe", t)])
                    xk = [("xsT", dk) for dk in range(16)]
                    for f in range(NF):
                        sg_ = load_w(w_g_d[ex_, f]); su_ = load_w(w_u_d[ex_, f])
                        bg_ = next_mm()
                        for k in range(16):
                            P.op("pe", lambda e, bg_=bg_, k=k, sg_=sg_: e.matmul(out=mm[bg_][:, 0:CSLOT], lhsT=wgu[sg_][:, k, :], rhs=xsT[:, k, :], start=(k == 0), stop=(k == 15)),
                                 reads=[("wgu", sg_)] + xk, writes=[("mm", bg_)], inc=(k == 15))
                        bu_ = next_mm()
                        for k in range(16):
                            P.op("pe", lambda e, bu_=bu_, k=k, su_=su_: e.matmul(out=mm[bu_][:, 0:CSLOT], lhsT=wgu[su_][:, k, :], rhs=xsT[:, k, :], start=(k == 0), stop=(k == 15)),
                                 reads=[("wgu", su_)] + xk, writes=[("mm", bu_)], inc=(k == 15))
                        c_ = cnt_[0] = cnt_[0] + 1
                        P.op("act", lambda e, bg_=bg_, c_=c_: e.activation(out=sg[c_ % 2][:], in_=mm[bg_][:, 0:CSLOT], func=AF.Silu), reads=[("mm", bg_)], writes=[("sg", c_ % 2)])
                        P.op("dve", lambda e, bu_=bu_, c_=c_, f=f: e.tensor_tensor(out=hid[:, f, :], in0=sg[c_ % 2][:], in1=mm[bu_][:, 0:CSLOT], op=ALU.mult),
                             reads=[("sg", c_ % 2), ("mm", bu_)], writes=[("hid", f)])
                    for sl in range(3):
                        sg3 = 3 * pi + sl
                        for half in range(2):
                            for k in range(8):
                                t = half * 8 + k
                                P.op("pe", lambda e, half=half, k=k, t=t, sl=sl: e.transpose(out=tp[:, half, k, :], in_=Pe[:, t, sl * 128:(sl + 1) * 128], identity=ident_b[:]),
                                     reads=[("Pe", t), "ident_b"], writes=[("tp", half)], inc=(k == 7))
                            evac2(PgT[:, sg3, half * 1024:(half + 1) * 1024].rearrange("p (k t) -> p k t", t=128), tp[:, half, :, :], ("tp", half), ("PgT", sg3))
                    for db in range(4):
                        ws = (ex_ * 4 + db) % 2
                        P.dma("pool", wd[ws][:], w_d_d[ex_, db].rearrange("p (k n) -> p k n", n=512), writes=[("wd", ws)])
                        for sl in range(3):
                            sg3 = 3 * pi + sl
                            b_ = next_mm()
                            for f in range(NF):
                                P.op("pe", lambda e, b_=b_, f=f, sl=sl, ws=ws: e.matmul(out=mm[b_][:], lhsT=hid[:, f, sl * 128:(sl + 1) * 128], rhs=wd[ws][:, f, :], start=(f == 0), stop=(f == NF - 1)),
                                     reads=[("hid", f), ("wd", ws)], writes=[("mm", b_)], inc=(f == NF - 1))
                            P.op("dve", lambda e, b_=b_, sg3=sg3, db=db: e.tensor_tensor(out=ye[:, sg3, db * 512:(db + 1) * 512], in0=mm[b_][:], in1=G2[:, db * 512:(db + 1) * 512], op=ALU.mult),
                                 reads=[("mm", b_), "G2"], writes=[("ye", sg3, db)])
                    if pi == 0:
                        continue
                    for ti in range(16):
                        for db in range(4):
                            b_ = next_mm()
                            for sl in range(6):
                                P.op("pe", lambda e, b_=b_, sl=sl, ti=ti, db=db: e.matmul(out=mm[b_][:], lhsT=PgT[:, sl, ti * 128:(ti + 1) * 128], rhs=ye[:, sl, db * 512:(db + 1) * 512], start=(sl == 0), stop=(sl == 5)),
                                     reads=[("PgT", sl), ("ye", sl, db)], writes=[("mm", b_)], inc=(sl == 5))
                            ys = ysl[0] = (ysl[0] + 1) % 3
                            evac2(yst[ys][:], mm[b_][:], ("mm", b_), ("yst", ys))
                            P.dma("pool", st_x1[ti * 128:(ti + 1) * 128, db * 512:(db + 1) * 512], yst[ys][:], reads=[("yst", ys)], writes=[("x1", ti, db)], accum_op=ALU.add)
            P.barrier()
            with contextlib.ExitStack() as s7:
                FG = sbt(s7, "FG", [128, D], F32)
                xt = [sbt(s7, f"xt{i}", [128, D], F32) for i in range(4)]
                ot = [sbt(s7, f"ot{i}", [128, D], F32) for i in range(4)]
                jk = sbt(s7, "jk", [128, D], BF16)
                P.dma("sp", FG[:], fg_d.partition_broadcast(128), writes=["FG"])
                for t in range(16):
                    xs = t % 4
                    i = stat_i[0] = (stat_i[0] + 1) % 40
                    sk = f"st{i}"
                    P.dma("sp", xt[xs][:], st_x1[t * 128:(t + 1) * 128, :], writes=[("xt", xs)])
                    ss = stats[:, 4 * i:4 * i + 1]
                    P.op("act", lambda e, xs=xs, ss=ss: e.activation(out=jk[:], in_=xt[xs][:], func=AF.Square, accum_out=ss), reads=[("xt", xs)], writes=["jk", sk])
                    rs, rk = rstd_from_ss(ss, D, sk)
                    P.op("dve", lambda e, xs=xs, rs=rs: e.scalar_tensor_tensor(out=ot[xs][:], in0=xt[xs][:], scalar=rs, in1=FG[:], op0=ALU.mult, op1=ALU.mult), reads=[("xt", xs), rk, "FG"], writes=[("ot", xs)])
                    P.dma("pool", out_d[t * 128:(t + 1) * 128, :], ot[xs][:], reads=[("ot", xs)], writes=[("out", t)])

        with contextlib.ExitStack() as s12:
            A1 = sbt(s12, "A1", [128, D], F32)
            SH1 = sbt(s12, "SH1", [128, D], F32)
            s12.A1, s12.SH1 = A1, SH1
            with contextlib.ExitStack() as s0:
                cpk = sbt(s0, "cpk", [128, 32], F32)
                csl = sbt(s0, "csl", [128, 32], F32)
                rep = sbt(s0, "rep", [128, 32, 128], BF16)
                n1g = sbt(s0, "n1g", [128, D], F32)
                n2g = sbt(s0, "n2g", [128, D], F32)
                wm = [sbt(s0, f"wm{i}", [128, 16, 512], BF16) for i in range(2)]
                bm = [sbt(s0, f"bm{i}", [128, 512], F32) for i in range(2)]
                stg = [sbt(s0, f"stg{i}", [128, 512], F32) for i in range(2)]
                tmpV = sbt(s0, "tmpV", [128, D], F32)
                P.dma("sp", cpk[:], c_pk, writes=["cpk"])
                P.dma("sp", n1g[:], n1g_d.partition_broadcast(128), writes=["n1g"])
                P.dma("sp", n2g[:], n2g_d.partition_broadcast(128), writes=["n2g"])
                P.op("act", lambda e: e.activation(out=csl[:], in_=cpk[:], func=AF.Silu), reads=["cpk"], writes=["csl"])
                for k in range(32):
                    P.op("dve", lambda e, k=k: e.tensor_scalar(out=rep[:, k, :], in0=ones_f[:], scalar1=csl[:, k:k + 1], scalar2=None, op0=ALU.mult),
                         reads=["csl", "ones_f"], writes=["rep"])
                for i in range(8):
                    s_ = i % 2
                    who = 0 if i < 6 else 1
                    P.dma("pool", wm[s_][:], w_mod_d[i].rearrange("p (k n) -> p k n", n=512), writes=[("wm", s_)])
                    P.dma("pool", bm[s_][:], b_mod_d[i:i + 1, :].partition_broadcast(128), writes=[("bm", s_)])
                    b_ = next_mm()
                    for k in range(16):
                        P.op("pe", lambda e, b_=b_, k=k, who=who, s_=s_: e.matmul(out=mm[b_][:], lhsT=rep[:, who * 16 + k, :], rhs=wm[s_][:, k, :], start=(k == 0), stop=(k == 15)),
                             reads=["rep", ("wm", s_)], writes=[("mm", b_)], inc=(k == 15))
                    P.op("dve", lambda e, b_=b_, s_=s_: e.tensor_tensor(out=stg[s_][:], in0=mm[b_][:], in1=bm[s_][:], op=ALU.add), reads=[("mm", b_), ("bm", s_)], writes=[("stg", s_)])
                    P.dma("sp", mod_part[i:i + 1, :], stg[s_][0:1, :], reads=[("stg", s_)], writes=[("mod_part", i)])
                P.coll(lambda e: e.collective_compute("AllGather", ALU.bypass, replica_groups=[[0, 1, 2, 3], [4, 5, 6, 7]], ins=[mod_part], outs=[mod_all]),
                       reads=[("mod_part", i) for i in range(8)], writes=["mod_all"])

                def sec_bcast(dst, sec, key):
                    for q_ in range(4):
                        P.dma("sp", dst[:, q_ * 512:(q_ + 1) * 512], mod_all[q_ * 8 + sec:q_ * 8 + sec + 1, :].partition_broadcast(128), reads=["mod_all"], writes=[key])

                def sec_copy(row, sec):
                    for q_ in range(4):
                        P.dma("sp", modrows[row:row + 1, q_ * 512:(q_ + 1) * 512], mod_all[q_ * 8 + sec:q_ * 8 + sec + 1, :], reads=["mod_all"], writes=[("modrow", row)])

                sec_bcast(SH1, 0, "SH1")
                sec_bcast(tmpV, 1, "tmpV")
                P.op("dve", lambda e: e.scalar_tensor_tensor(out=A1[:], in0=tmpV[:], scalar=1.0, in1=n1g[:], op0=ALU.add, op1=ALU.mult), reads=["tmpV", "n1g"], writes=["A1"])
                sec_copy(0, 2)
                sec_copy(2, 3)
                sec_copy(3, 5)
                sec_copy(5, 6)
                sec_bcast(tmpV, 4, "tmpV")
                P.op("dve", lambda e: e.scalar_tensor_tensor(out=tmpV[:], in0=tmpV[:], scalar=1.0, in1=n2g[:], op0=ALU.add, op1=ALU.mult), reads=["tmpV", "n2g"], writes=["tmpV"])
                P.dma("sp", modrows[1:2, :], tmpV[0:1, :], reads=["tmpV"], writes=[("modrow", 1)])
                sec_bcast(tmpV, 7, "tmpV")
                P.op("dve", lambda e: e.scalar_tensor_tensor(out=tmpV[:], in0=tmpV[:], scalar=1.0, in1=n1g[:], op0=ALU.add, op1=ALU.mult), reads=["tmpV", "n1g"], writes=["tmpV"])
                P.dma("sp", modrows[4:5, :], tmpV[0:1, :], reads=["tmpV"], writes=[("modrow", 4)])
            if dbg:
                P.dma("sp", dbgout["d_modrows"], modrows, reads=[("modrow", r_) for r_ in range(6)], writes=["d_modrows"])
            P.barrier()
            if stop_after >= 1:
                with contextlib.ExitStack() as s1:
                    xnT = sbt(s1, "xnT", [128, 16, NOWN + 128], BF16)
                    with contextlib.ExitStack() as s1a:
                        xt = [sbt(s1a, f"xt{i}", [128, D], F32) for i in range(4)]
                        hb = [sbt(s1a, f"hb{i}", [128, D], BF16) for i in range(4)]
                        for t in range(17):
                            s_ = t % 4
                            if t < 16:
                                src = x_own[t * 128:(t + 1) * 128, :]
                            else:
                                P.op("pool", lambda e, s_=s_: e.memset(xt[s_][:], 0.0), writes=[("xt", s_)])
                                P.dma("sp", xt[s_][0:2, :], x_halo, writes=[("xt", s_)])
                                src = None
                            tok_pipeline(xt[s_], ("xt", s_), hb[s_], ("hb", s_), A1, "A1", SH1, "SH1",
                                         lambda half, t=t: xnT[:, half * 8:(half + 1) * 8, t * 128:(t + 1) * 128], ("xnT", t // 4), src=src)
                    P.barrier()
                    with contextlib.ExitStack() as s1b:
                        wch = [sbt(s1b, f"wch{i}", [128, 16, 128], BF16) for i in range(6)]
                        wkv = sbt(s1b, "wkv", [128, 16, 640], BF16)
                        xin_sb = [sbt(s1b, f"xin_sb{i}", [128, 512], F32) for i in range(2)]
                        Ubuf = sbt(s1b, "Ubuf", [128, NOWN + 2], F32)
                        Uh = sbt(s1b, "Uh", [128, 128], F32)
                        bg_sb = sbt(s1b, "bg_sb", [128, NOWN], F32)
                        Tc = sbt(s1b, "Tc", [128, NOWN], F32)
                        yc = [sbt(s1b, f"yc{i}", [128, NOWN], BF16) for i in range(2)]
                        cw = sbt(s1b, "cw", [128, 24], F32)
                        hmask = sbt(s1b, "hmask", [128, 2], F32)
                        qg = sbt(s1b, "qg", [128, 4], F32)
                        kvg = sbt(s1b, "kvg", [128, 4], F32)
                        raw = sbt(s1b, "raw", [128, 4, 512], F32)
                        sq = sbt(s1b, "sq", [128, 4, 512], BF16)
                        rr = sbt(s1b, "rr", [128, 512], F32)
                        lat = [sbt(s1b, f"lat{i}", [128, 4, 512], BF16) for i in range(2)]
                        rt1 = sbt(s1b, "rt1", [64, 512], F32)
                        rt2 = sbt(s1b, "rt2", [64, 512], F32)
                        cst = sbt(s1b, "cst", [64, 2, 512], F32)
                        krs = [sbt(s1b, f"krs{i}", [64, 512], BF16) for i in range(2)]
                        P.dma("sp", cw[:], conv_d, writes=["cw"])
                        P.dma("sp", hmask[:], hmask_d, writes=["hmask"])
                        P.dma("sp", qg[:], qg_d, writes=["gam"])
                        P.dma("sp", kvg[:], kvg_d, writes=["gam"])
                        for i in range(5):
                            P.dma("pool", wkv[:, :, i * 128:(i + 1) * 128], w_in_d[28 + i].rearrange("p (k n) -> p k n", n=128), writes=["wkv"])
                        wslot = [0]

                        def load_chunk(c):
                            s_ = wslot[0] = (wslot[0] + 1) % 6
                            P.dma("pool", wch[s_][:], w_in_d[c].rearrange("p (k n) -> p k n", n=128), writes=[("wch", s_)])
                            return s_

                        groups = [(0, 512), (512, 512), (1024, 512), (1536, 512), (2048, 128)]
                        for gi in range(4):
                            g0 = gi * 512
                            lt = lat[gi % 2]
                            latent_norm(lambda ci, k: wkv[:, k, ci * 128:(ci + 1) * 128], lambda k, g0=g0: xnT[:, k, g0:g0 + 512],
                                        ["wkv", ("xnT", gi)], kvg, 512,
                                        lambda ci, lt=lt: lt[:, ci, :], ("lat", gi % 2), (raw, sq, rr))
                            for c_ in range(4):
                                P.dma("sp", st_ckvn_l[c_][:, g0:g0 + 512], lt[:, c_, :], reads=[("lat", gi % 2)], writes=[("st_ckvn", gi, c_)])
                            P.dma("sp", cst[:, 0, :], cos_own_d[:, g0:g0 + 512], writes=["cst"])
                            P.dma("sp", cst[:, 1, :], sin_own_d[:, g0:g0 + 512], writes=["cst"])
                            rope_proj(lambda k: wkv[:, k, 512:576], lambda k: wkv[:, k, 576:640], lambda k, g0=g0: xnT[:, k, g0:g0 + 512],
                                      ["wkv", ("xnT", gi)], 16, 512, cst[:, 0, :], cst[:, 1, :], "cst", krs[gi % 2][:], ("krs", gi % 2), (rt1, rt2))
                            P.dma("sp", st_kr[:, g0:g0 + 512], krs[gi % 2][:], reads=[("krs", gi % 2)], writes=[("st_kr", gi)])
                        RG = [[0, 1, 2, 3], [4, 5, 6, 7]]
                        for c_ in range(4):
                            P.coll(lambda e, c_=c_: e.collective_compute("AllGather", ALU.bypass, replica_groups=RG, ins=[st_ckvn_l[c_]], outs=[ag_ck_l[c_]]),
                                   reads=[("st_ckvn", gi, c_) for gi in range(4)], writes=[("ag_ck", c_)])
                        P.coll(lambda e: e.collective_compute("AllGather", ALU.bypass, replica_groups=RG, ins=[st_kr], outs=[ag_kr]),
                               reads=[("st_kr", gi) for gi in range(4)], writes=["ag_kr"])
                        for j in range(8):
                            sx, sb_, sc_ = load_chunk(j), load_chunk(8 + j), load_chunk(16 + j)
                            for gi, (g0, gn) in enumerate(groups):
                                xk = ("xnT", min(gi, 4))
                                ba = next_mm()
                                for k in range(16):
                                    P.op("pe", lambda e, ba=ba, k=k, g0=g0, gn=gn, sx=sx: e.matmul(out=mm[ba][:, 0:gn], lhsT=wch[sx][:, k, :], rhs=xnT[:, k, g0:g0 + gn], start=(k == 0), stop=(k == 15)),
                                         reads=[("wch", sx), xk], writes=[("mm", ba)], inc=(k == 15))
                                xs = xin_sb[gi % 2]
                                P.op("act", lambda e, ba=ba, gn=gn, xs=xs: e.copy(out=xs[:, 0:gn], in_=mm[ba][:, 0:gn]), reads=[("mm", ba)], writes=[("xin_sb", gi % 2)])
                                bc_ = next_mm()
                                for k in range(16):
                                    P.op("pe", lambda e, bc_=bc_, k=k, g0=g0, gn=gn, sc_=sc_: e.matmul(out=mm[bc_][:, 0:gn], lhsT=wch[sc_][:, k, :], rhs=xnT[:, k, g0:g0 + gn], start=(k == 0), stop=(k == 15)),
                                         reads=[("wch", sc_), xk], writes=[("mm", bc_)], inc=(k == 15))
                                if gi < 4:
                                    P.op("dve", lambda e, bc_=bc_, g0=g0, xs=xs: e.tensor_tensor(out=Ubuf[:, 1 + g0:1 + g0 + 512], in0=mm[bc_][:], in1=xs[:], op=ALU.mult),
                                         reads=[("mm", bc_), ("xin_sb", gi % 2)], writes=["Ubuf"])
                                    bb = next_mm()
                                    for k in range(16):
                                        P.op("pe", lambda e, bb=bb, k=k, g0=g0, sb_=sb_: e.matmul(out=mm[bb][:], lhsT=wch[sb_][:, k, :], rhs=xnT[:, k, g0:g0 + 512], start=(k == 0), stop=(k == 15)),
                                             reads=[("wch", sb_), xk], writes=[("mm", bb)], inc=(k == 15))
                                    P.op("act", lambda e, bb=bb, g0=g0: e.copy(out=bg_sb[:, g0:g0 + 512], in_=mm[bb][:]), reads=[("mm", bb)], writes=["bg_sb"])
                                else:
                                    P.op("dve", lambda e, bc_=bc_, xs=xs: e.tensor_tensor(out=Uh[:], in0=mm[bc_][:, 0:128], in1=xs[:, 0:128], op=ALU.mult),
                                         reads=[("mm", bc_), ("xin_sb", gi % 2)], writes=["Uh"])
                            P.op("pool", lambda e: e.tensor_tensor(out=Ubuf[:, 0:1], in0=Uh[:, 0:1], in1=hmask[:, 0:1], op=ALU.mult), reads=["Uh", "hmask"], writes=["Ubuf"])
                            P.op("pool", lambda e: e.tensor_tensor(out=Ubuf[:, NOWN + 1:NOWN + 2], in0=Uh[:, 1:2], in1=hmask[:, 1:2], op=ALU.mult), reads=["Uh", "hmask"], writes=["Ubuf"])
                            P.op("dve", lambda e, j=j: e.tensor_scalar(out=Tc[:], in0=Ubuf[:, 0:NOWN], scalar1=cw[:, 3 * j:3 * j + 1], scalar2=None, op0=ALU.mult), reads=["Ubuf", "cw"], writes=["Tc"])
                            P.op("dve", lambda e, j=j: e.scalar_tensor_tensor(out=Tc[:], in0=Ubuf[:, 1:NOWN + 1], scalar=cw[:, 3 * j + 1:3 * j + 2], in1=Tc[:], op0=ALU.mult, op1=ALU.add), reads=["Ubuf", "cw", "Tc"], writes=["Tc"])
                            P.op("dve", lambda e, j=j: e.scalar_tensor_tensor(out=Tc[:], in0=Ubuf[:, 2:NOWN + 2], scalar=cw[:, 3 * j + 2:3 * j + 3], in1=Tc[:], op0=ALU.mult, op1=ALU.add), reads=["Ubuf", "cw", "Tc"], writes=["Tc"])
                            P.op("dve", lambda e, j=j: e.tensor_tensor(out=yc[j % 2][:], in0=Tc[:], in1=bg_sb[:], op=ALU.mult), reads=["Tc", "bg_sb"], writes=[("yc", j % 2)])
                            P.dma("sp", st_ymix[:, j * NOWN:(j + 1) * NOWN], yc[j % 2][:], reads=[("yc", j % 2)], writes=[("st_ymix", j)])
                        qs = [load_chunk(24 + ci) for ci in range(4)]
                        for gi in range(4):
                            g0 = gi * 512
                            lt = lat[gi % 2]
                            latent_norm(lambda ci, k: wch[qs[ci]][:, k, :], lambda k, g0=g0: xnT[:, k, g0:g0 + 512],
                                        [("wch", s_) for s_ in qs] + [("xnT", gi)], qg, 512,
                                        lambda ci, lt=lt: lt[:, ci, :], ("lat", gi % 2), (raw, sq, rr))
                            P.dma("sp", st_cqn.rearrange("p (c n) -> p c n", n=NOWN)[:, :, g0:g0 + 512], lt[:], reads=[("lat", gi % 2)], writes=[("st_cqn", gi)])
                P.barrier()
            if dbg:
                P.dma("sp", dbgout["d_cqn"], st_cqn, reads=[("st_cqn", g) for g in range(4)], writes=["d_cqn"])
                P.dma("sp", dbgout["d_ymix"], st_ymix, writes=["d_ymix"]) if stop_after < 3 else None
            if stop_after >= 2:
                phase23(s12)
        if stop_after >= 4:
            phase4()
        if stop_after >= 6:
            phase6()
        P.barrier()
        P.run()
    return nc


def _rope_tables(tok):
    tok = np.asarray(tok)
    row = (tok // 64).astype(np.float32)
    col = (tok % 64).astype(np.float32)
    inv = (np.float32(10000.0) ** (-np.arange(16, dtype=np.float32) / np.float32(16))).astype(np.float32)
    ar = row[None, :] * inv[:, None]
    ac = col[None, :] * inv[:, None]
    cos = np.concatenate([np.cos(ar), np.cos(ar), np.cos(ac), np.cos(ac)], 0).astype(np.float32)
    sin = np.concatenate([-np.sin(ar), np.sin(ar), -np.sin(ac), np.sin(ac)], 0).astype(np.float32)
    return np.ascontiguousarray(cos), np.ascontiguousarray(sin)


_SWAP = np.concatenate([np.arange(16, 32), np.arange(0, 16), np.arange(48, 64), np.arange(32, 48)])


def _prep_shared(inp):
    f = lambda a: np.ascontiguousarray(a, dtype=np.float32)
    sh = {}
    w_mod = inp["w_mod"][0]
    w_mod_r = w_mod.reshape(16, 128, 24, 512).transpose(2, 1, 0, 3).reshape(24, 128, 16 * 512)
    b_mod_r = inp["b_mod"][0].reshape(24, 512)
    sh["_w_mod_q"] = [f(w_mod_r[[q + 4 * i for i in range(6)] + [q, q + 4]]) for q in range(4)]
    sh["_b_mod_q"] = [f(b_mod_r[[q + 4 * i for i in range(6)] + [q, q + 4]]) for q in range(4)]
    sh["norm1_g"] = f(inp["norm1_g"][0][None]); sh["norm2_g"] = f(inp["norm2_g"][0][None]); sh["final_g"] = f(inp["final_g"][None])
    w_in = inp["w_in"][0]
    w_ext = np.concatenate([w_in, w_in[:, 4096:][:, _SWAP]], 1)
    sh["w_in_r"] = f(w_ext.reshape(16, 128, 33, 128).transpose(2, 1, 0, 3).reshape(33, 128, 16 * 128))
    sh["conv_r"] = f(inp["conv_w"][0].reshape(3, 8, 128).transpose(2, 1, 0).reshape(128, 24))
    sh["qg_r"] = f(inp["q_norm_g"][0].reshape(4, 128).T); sh["kvg_r"] = f(inp["kv_norm_g"][0].reshape(4, 128).T)
    w_uq = inp["w_uq"][0]
    w_uq_e = np.concatenate([w_uq, w_uq[:, :, 128:][:, :, _SWAP]], 2)
    sh["w_uq_r"] = f(w_uq_e.reshape(4, 128, 8, 256).transpose(2, 1, 0, 3).reshape(8, 128, 4 * 256))
    sh["w_ukv_r"] = f(inp["w_ukv"][0].reshape(4, 128, 8, 256).transpose(2, 1, 0, 3).reshape(8, 128, 4 * 256))
    sh["w_out_r"] = f(inp["w_out"][0].reshape(16, 128, D).transpose(1, 0, 2).reshape(128, 16 * D))
    sh["w_rt_r"] = f(inp["w_router"][0].reshape(16, 128, NE).transpose(1, 0, 2).reshape(128, 16 * NE))
    sh["w_g_r"] = f(inp["w_gate"][0].reshape(NE, 16, 128, NF, 128).transpose(0, 3, 2, 1, 4).reshape(NE, NF, 128, 16 * 128))
    sh["w_u_r"] = f(inp["w_up"][0].reshape(NE, 16, 128, NF, 128).transpose(0, 3, 2, 1, 4).reshape(NE, NF, 128, 16 * 128))
    sh["w_d_r"] = f(inp["w_down"][0].reshape(NE, NF, 128, 4, 512).transpose(0, 3, 2, 1, 4).reshape(NE, 4, 128, NF * 512))
    sh["ident"] = np.eye(128, dtype=np.float32)
    sh["gmat"] = f((np.arange(128)[:, None] % 16) == (np.arange(128)[None, :] % 16))
    sh["ltri"] = f(np.arange(128)[:, None] < np.arange(128)[None, :])
    sh["iota"] = f(np.tile(np.arange(CSLOT, dtype=np.float32)[None], (128, 1)))
    return sh


def _prep_core(inp, r):
    f = lambda a: np.ascontiguousarray(a, dtype=np.float32)
    b, q = r // 4, r % 4
    t0 = q * NOWN
    x = inp["x"][b]
    m = {}
    m["x_own"] = f(x[t0:t0 + NOWN])
    m["x_ctx"] = f(inp["ctx"][b])
    m["x_halo"] = f(np.stack([x[max(t0 - 1, 0)], x[min(t0 + NOWN, 8191)]], 0))
    m["hmask"] = f(np.tile(np.array([[1.0 if q > 0 else 0.0, 1.0 if q < 3 else 0.0]], np.float32), (128, 1)))
    m["c_pk"] = f(np.concatenate([inp["c"][b].reshape(16, 128).T, inp["c_ctx"].reshape(16, 128).T], 1))
    tok_own = np.arange(t0, t0 + NOWN)
    m["cos_own"], m["sin_own"] = _rope_tables(tok_own)
    return m


def kernel(**inputs):
    inp = {k: np.asarray(v) for k, v in inputs.items()}
    nc = build()
    sh = _prep_shared(inp)
    in_maps = []
    for r in range(8):
        m = {k: v for k, v in sh.items() if not k.startswith("_")}
        m["w_mod_q"] = sh["_w_mod_q"][r % 4]
        m["b_mod_q"] = sh["_b_mod_q"][r % 4]
        m.update(_prep_core(inp, r))
        in_maps.append(m)
    res = run_bass_kernel_spmd(nc, in_maps, core_ids=list(range(8)))
    out = np.empty((2, 8192, D), np.float32)
    for r in range(8):
        b, q = r // 4, r % 4
        out[b, q * NOWN:(q + 1) * NOWN] = res.results[r]["out"]
    return out
```
